# Optimizing a Trainium2 kernel written in Bass

```python
import math
import jax
import jax.numpy as jnp
from jax import lax

D_MODEL = 2048
BATCH = 4
SEQ = 8192
DEPTH = 2

GRID_W = 64
CTX_LEN = 256
ROPE_BASE = 10000.0
NORM_EPS = 1e-6
NEG_INF = -1e30
Q_BLOCK = 128

POOL_GROUPS = 4
POOL_WINDOWS = (2, 4, 8, 16)
POOL_GROUP_DIM = D_MODEL // 8
POOL_WIDTH = POOL_GROUPS * POOL_GROUP_DIM
MLA_HEADS = 8
MLA_NOPE = 128
MLA_ROPE = 64
MLA_V = 128
MLA_KV_RANK = D_MODEL // 4
MLA_SCALE = (MLA_NOPE + MLA_ROPE) ** -0.5
WIN_HEADS = 8
WIN_KV_HEADS = 2
WIN_HEAD_DIM = 128
WINDOW = 128
WIN_SCALE = WIN_HEAD_DIM ** -0.5
DIFF_HEADS = 4
DIFF_HEAD_DIM = 128
DIFF_SCALE = DIFF_HEAD_DIM ** -0.5

N_BRANCHES = 4
D_FF = D_MODEL * 7 // 2
N_EXPERTS = 8
TOP_K = 2
EXPERT_FF = D_MODEL * 7 // 2

IN_SPLITS = (POOL_WIDTH,
             MLA_HEADS * (MLA_NOPE + MLA_ROPE), MLA_KV_RANK, MLA_ROPE,
             WIN_HEADS * WIN_HEAD_DIM, WIN_KV_HEADS * WIN_HEAD_DIM, WIN_KV_HEADS * WIN_HEAD_DIM,
             DIFF_HEADS * 2 * DIFF_HEAD_DIM, DIFF_HEADS * 2 * DIFF_HEAD_DIM, DIFF_HEADS * 2 * DIFF_HEAD_DIM,
             N_BRANCHES * D_MODEL)
IN_COLS = sum(IN_SPLITS)

kernel_name = 'hybrid_pool_mla_swa_diff_moe_dit'


def _rmsnorm(x, g):
    xf = x.astype(jnp.float32)
    y = xf * lax.rsqrt(jnp.mean(xf * xf, axis=-1, keepdims=True) + NORM_EPS)
    return (y * g.astype(jnp.float32)).astype(x.dtype)


def _modulate(x, g, shift, scale):
    return _rmsnorm(x, g) * (1.0 + scale) + shift


def _rotate_1d(x, pos, dim):
    half = dim // 2
    inv_freq = ROPE_BASE ** (-jnp.arange(half, dtype=jnp.float32) / half)
    ang = pos.astype(jnp.float32)[:, None] * inv_freq[None, :]
    shape = (pos.shape[0],) + (1,) * (x.ndim - 3) + (half,)
    cos = jnp.cos(ang).reshape(shape)
    sin = jnp.sin(ang).reshape(shape)
    xf = x.astype(jnp.float32)
    x1, x2 = xf[..., :half], xf[..., half:]
    return jnp.concatenate([x1 * cos - x2 * sin, x2 * cos + x1 * sin], axis=-1).astype(x.dtype)


def _axial_rope(x, rows, cols):
    h = x.shape[-1] // 2
    return jnp.concatenate([_rotate_1d(x[..., :h], rows, h), _rotate_1d(x[..., h:], cols, h)], axis=-1)


def _split_in(z):
    idx, acc = [], 0
    for s in IN_SPLITS[:-1]:
        acc += s
        idx.append(acc)
    return jnp.split(z, idx, axis=-1)


def _centred_mean_minus_self(x, w):
    n = x.shape[1]
    xf = x.astype(jnp.float32)
    csum = jnp.pad(lax.cumsum(xf, axis=1), ((0, 0), (1, 0), (0, 0)))
    t = jnp.arange(n)
    lo = jnp.clip(t - w // 2, 0, n)
    hi = jnp.clip(t - w // 2 + w, 0, n)
    s = jnp.take(csum, hi, axis=1) - jnp.take(csum, lo, axis=1)
    cnt = (hi - lo).astype(jnp.float32)
    return (s / cnt[None, :, None] - xf).astype(x.dtype)


def _pool_mixer(a, lp):
    bsz, n, _ = a.shape
    ag = a.reshape(bsz, n, POOL_GROUPS, POOL_GROUP_DIM)
    pooled = jnp.stack([_centred_mean_minus_self(ag[:, :, i], w) for i, w in enumerate(POOL_WINDOWS)], axis=2)
    mixed = jnp.einsum('bngc,gcd->bngd', pooled, lp['pool_w']).reshape(bsz, n, POOL_WIDTH)
    return (mixed * lp['pool_scale']) @ lp['pool_out']


def _project(u, lp, pos):
    bsz, n, _ = u.shape
    a_in, q_b, ckv, kr, q_c, k_c, v_c, q_d, k_d, v_d, gate_logits = _split_in(u @ lp['w_in'])
    if pos is None:
        rope = lambda t: t
    else:
        rope = lambda t: _axial_rope(t, pos[0], pos[1])
    q_b = q_b.reshape(bsz, n, MLA_HEADS, MLA_NOPE + MLA_ROPE)
    kv = (_rmsnorm(ckv, lp['mla_kv_norm_g']) @ lp['mla_w_kv_b']).reshape(bsz, n, MLA_HEADS, MLA_NOPE + MLA_V)
    k_rope = jnp.broadcast_to(rope(kr[:, :, None, :]), (bsz, n, MLA_HEADS, MLA_ROPE))
    return {
        'pool': a_in,
        'mla_q': jnp.concatenate([q_b[..., :MLA_NOPE], rope(q_b[..., MLA_NOPE:])], axis=-1),
        'mla_k': jnp.concatenate([kv[..., :MLA_NOPE], k_rope], axis=-1),
        'mla_v': kv[..., MLA_NOPE:],
        'win_q': rope(q_c.reshape(bsz, n, WIN_HEADS, WIN_HEAD_DIM)),
        'win_k': rope(k_c.reshape(bsz, n, WIN_KV_HEADS, WIN_HEAD_DIM)),
        'win_v': v_c.reshape(bsz, n, WIN_KV_HEADS, WIN_HEAD_DIM),
        'diff_q': rope(q_d.reshape(bsz, n, DIFF_HEADS, 2, DIFF_HEAD_DIM)),
        'diff_k': rope(k_d.reshape(bsz, n, DIFF_HEADS, 2, DIFF_HEAD_DIM)),
        'diff_v': v_d.reshape(bsz, n, DIFF_HEADS, 2 * DIFF_HEAD_DIM),
        'gates': jax.nn.sigmoid(gate_logits.reshape(bsz, n, N_BRANCHES, D_MODEL)),
    }


def _dense_attn(q, k, v, scale):
    b, sq, h, dk = q.shape
    nb = sq // Q_BLOCK
    qb = jnp.moveaxis(q.reshape(b, nb, Q_BLOCK, h, dk), 1, 0)

    def block(qi):
        s = jnp.einsum('bqhd,bkhd->bhqk', qi, k).astype(jnp.float32) * scale
        p = jax.nn.softmax(s, axis=-1).astype(v.dtype)
        return jnp.einsum('bhqk,bkhd->bqhd', p, v)

    o = lax.map(block, qb)
    return jnp.moveaxis(o, 0, 1).reshape(b, sq, h, v.shape[-1])


def _diff_attn(q, k, v, lam, scale):
    b, sq, h, _, dk = q.shape
    nb = sq // Q_BLOCK
    qb = jnp.moveaxis(q.reshape(b, nb, Q_BLOCK, h, 2, dk), 1, 0)

    def block(qi):
        s = jnp.einsum('bqhmd,bkhmd->bhmqk', qi, k).astype(jnp.float32) * scale
        p = jax.nn.softmax(s, axis=-1)
        p = (p[:, :, 0] - lam * p[:, :, 1]).astype(v.dtype)
        return jnp.einsum('bhqk,bkhd->bqhd', p, v)

    o = lax.map(block, qb)
    return jnp.moveaxis(o, 0, 1).reshape(b, sq, h, v.shape[-1])


def _window_attn(q, k, v, kc, vc, sink, scale):
    b, n, hq, d = q.shape
    hkv = k.shape[2]
    g = hq // hkv
    nb = n // Q_BLOCK
    span = Q_BLOCK + 2 * WINDOW
    ctx_len = kc.shape[1]
    padw = ((0, 0), (WINDOW, WINDOW), (0, 0), (0, 0))
    kp = jnp.pad(k, padw)
    vp = jnp.pad(v, padw)
    qb = jnp.moveaxis(q.reshape(b, nb, Q_BLOCK, hkv, g, d), 1, 0)
    sink_l = jnp.broadcast_to(sink.astype(jnp.float32).reshape(1, hkv, g, 1, 1), (b, hkv, g, Q_BLOCK, 1))
    rel = jnp.arange(span)[None, :] - WINDOW - jnp.arange(Q_BLOCK)[:, None]
    in_band = jnp.abs(rel) <= WINDOW

    def block(args):
        i, qi = args
        start = i * Q_BLOCK
        kw = lax.dynamic_slice_in_dim(kp, start, span, axis=1)
        vw = lax.dynamic_slice_in_dim(vp, start, span, axis=1)
        kpos = start - WINDOW + jnp.arange(span)
        valid = in_band & ((kpos >= 0) & (kpos < n))[None, :]
        s_loc = jnp.einsum('bqhgd,bkhd->bhgqk', qi, kw).astype(jnp.float32) * scale
        s_loc = jnp.where(valid, s_loc, NEG_INF)
        s_ctx = jnp.einsum('bqhgd,bkhd->bhgqk', qi, kc).astype(jnp.float32) * scale
        p = jax.nn.softmax(jnp.concatenate([s_loc, s_ctx, sink_l], axis=-1), axis=-1).astype(v.dtype)
        return (jnp.einsum('bhgqk,bkhd->bqhgd', p[..., :span], vw)
                + jnp.einsum('bhgqk,bkhd->bqhgd', p[..., span:span + ctx_len], vc))

    o = lax.map(block, (jnp.arange(nb), qb))
    return jnp.moveaxis(o, 0, 1).reshape(b, n, hq, d)


def _sink_attn(q, k, v, sink, scale):
    b, l, hq, d = q.shape
    hkv = k.shape[2]
    g = hq // hkv
    qg = q.reshape(b, l, hkv, g, d)
    s = jnp.einsum('bqhgd,bkhd->bhgqk', qg, k).astype(jnp.float32) * scale
    sk = jnp.broadcast_to(sink.astype(jnp.float32).reshape(1, hkv, g, 1, 1), (b, hkv, g, l, 1))
    p = jax.nn.softmax(jnp.concatenate([s, sk], axis=-1), axis=-1)[..., :l].astype(v.dtype)
    return jnp.einsum('bhgqk,bkhd->bqhgd', p, v).reshape(b, l, hq, d)


def _diff_lambda(lam_params, lam_init):
    lp = lam_params.astype(jnp.float32)
    return jnp.exp(jnp.sum(lp[0] * lp[1])) - jnp.exp(jnp.sum(lp[2] * lp[3])) + lam_init


def _merge(y_pool, mla_o, win_o, diff_o, gates, lp, lam_init):
    bsz, n = y_pool.shape[:2]
    y_mla = mla_o.reshape(bsz, n, MLA_HEADS * MLA_V) @ lp['mla_out']
    y_win = win_o.reshape(bsz, n, WIN_HEADS * WIN_HEAD_DIM) @ lp['win_out']
    diff_o = _rmsnorm(diff_o, lp['diff_subln_g']) * (1.0 - lam_init)
    y_diff = diff_o.reshape(bsz, n, DIFF_HEADS * 2 * DIFF_HEAD_DIM) @ lp['diff_out']
    merged = (gates[:, :, 0] * y_pool + gates[:, :, 1] * y_mla
              + gates[:, :, 2] * y_win + gates[:, :, 3] * y_diff)
    return merged @ lp['w_out']


def _mixer_sublayer(u, uc, pos, lp, lam_init, need_ctx):
    P = _project(u, lp, pos)
    C = _project(uc, lp, None)
    lam = _diff_lambda(lp['diff_lambda'], lam_init)
    y_pool = _pool_mixer(P['pool'], lp)
    mla_o = _dense_attn(P['mla_q'], jnp.concatenate([P['mla_k'], C['mla_k']], axis=1),
                        jnp.concatenate([P['mla_v'], C['mla_v']], axis=1), MLA_SCALE)
    win_o = _window_attn(P['win_q'], P['win_k'], P['win_v'], C['win_k'], C['win_v'], lp['win_sink'], WIN_SCALE)
    diff_o = _diff_attn(P['diff_q'], jnp.concatenate([P['diff_k'], C['diff_k']], axis=1),
                        jnp.concatenate([P['diff_v'], C['diff_v']], axis=1), lam, DIFF_SCALE)
    y = _merge(y_pool, mla_o, win_o, diff_o, P['gates'], lp, lam_init)
    if not need_ctx:
        return y, None
    yc_pool = _pool_mixer(C['pool'], lp)
    mla_c = _dense_attn(C['mla_q'], C['mla_k'], C['mla_v'], MLA_SCALE)
    win_c = _sink_attn(C['win_q'], C['win_k'], C['win_v'], lp['win_sink'], WIN_SCALE)
    diff_c = _diff_attn(C['diff_q'], C['diff_k'], C['diff_v'], lam, DIFF_SCALE)
    yc = _merge(yc_pool, mla_c, win_c, diff_c, C['gates'], lp, lam_init)
    return y, yc


def _swiglu(x, wg, wu, wd):
    return (jax.nn.silu(x @ wg) * (x @ wu)) @ wd


def _moe(x, router, router_b, wg, wu, wd):
    logits = (x @ router + router_b).astype(jnp.float32)
    top_v, top_i = lax.top_k(logits, TOP_K)
    top_w = jax.nn.softmax(top_v, axis=-1)
    gates = jnp.einsum('bnk,bnke->bne', top_w, jax.nn.one_hot(top_i, N_EXPERTS, dtype=jnp.float32)).astype(x.dtype)
    y = jnp.zeros_like(x)
    for e in range(N_EXPERTS):
        y = y + gates[..., e:e + 1] * _swiglu(x, wg[e], wu[e], wd[e])
    return y


def setup_inputs(seed: int = 0) -> dict:
    key = jax.random.key(seed)
    keys = jax.random.split(key, 32)
    f32 = jnp.float32
    D = D_MODEL
    n_dense = (DEPTH + 1) // 2
    n_moe = DEPTH // 2

    def nrm(i, shape, scale):
        return jax.random.normal(keys[i], shape, f32) * scale

    return {
        'x': nrm(0, (BATCH, SEQ, D), 1.0),
        'c': nrm(1, (BATCH, D), 1.0),
        'ctx': nrm(2, (BATCH, CTX_LEN, D), 1.0),
        'c_ctx': nrm(3, (D,), 1.0),
        'w_mod': nrm(4, (DEPTH, D, 6 * D), 0.5 * D ** -0.5),
        'b_mod': nrm(5, (DEPTH, 6 * D), 0.02),
        'norm1_g': 1.0 + nrm(6, (DEPTH, D), 0.02),
        'norm2_g': 1.0 + nrm(7, (DEPTH, D), 0.02),
        'w_in': nrm(8, (DEPTH, D, IN_COLS), D ** -0.5),
        'pool_w': nrm(9, (DEPTH, POOL_GROUPS, POOL_GROUP_DIM, POOL_GROUP_DIM), POOL_GROUP_DIM ** -0.5),
        'pool_scale': 1.0 + nrm(10, (DEPTH, POOL_WIDTH), 0.1),
        'pool_out': nrm(11, (DEPTH, POOL_WIDTH, D), POOL_WIDTH ** -0.5),
        'mla_kv_norm_g': 1.0 + nrm(12, (DEPTH, MLA_KV_RANK), 0.02),
        'mla_w_kv_b': nrm(13, (DEPTH, MLA_KV_RANK, MLA_HEADS * (MLA_NOPE + MLA_V)), MLA_KV_RANK ** -0.5),
        'mla_out': nrm(14, (DEPTH, MLA_HEADS * MLA_V, D), (MLA_HEADS * MLA_V) ** -0.5),
        'win_sink': nrm(15, (DEPTH, WIN_HEADS), 0.5),
        'win_out': nrm(16, (DEPTH, WIN_HEADS * WIN_HEAD_DIM, D), (WIN_HEADS * WIN_HEAD_DIM) ** -0.5),
        'diff_lambda': nrm(17, (DEPTH, 4, DIFF_HEAD_DIM), 0.1),
        'diff_subln_g': 1.0 + nrm(18, (DEPTH, 2 * DIFF_HEAD_DIM), 0.02),
        'diff_out': nrm(19, (DEPTH, DIFF_HEADS * 2 * DIFF_HEAD_DIM, D), (DIFF_HEADS * 2 * DIFF_HEAD_DIM) ** -0.5),
        'w_out': nrm(20, (DEPTH, D, D), D ** -0.5),
        'ffn_w_gate': nrm(21, (n_dense, D, D_FF), D ** -0.5),
        'ffn_w_up': nrm(22, (n_dense, D, D_FF), D ** -0.5),
        'ffn_w_down': nrm(23, (n_dense, D_FF, D), D_FF ** -0.5),
        'moe_router': nrm(24, (n_moe, D, N_EXPERTS), D ** -0.5),
        'moe_router_b': nrm(25, (n_moe, N_EXPERTS), 0.01),
        'moe_w_gate': nrm(26, (n_moe, N_EXPERTS, D, EXPERT_FF), D ** -0.5),
        'moe_w_up': nrm(27, (n_moe, N_EXPERTS, D, EXPERT_FF), D ** -0.5),
        'moe_w_down': nrm(28, (n_moe, N_EXPERTS, EXPERT_FF, D), EXPERT_FF ** -0.5),
        'final_norm_g': 1.0 + nrm(29, (D,), 0.02),
    }


def reference(x, c, ctx, c_ctx, w_mod, b_mod, norm1_g, norm2_g, w_in, pool_w, pool_scale, pool_out,
              mla_kv_norm_g, mla_w_kv_b, mla_out, win_sink, win_out, diff_lambda, diff_subln_g,
              diff_out, w_out, ffn_w_gate, ffn_w_up, ffn_w_down, moe_router, moe_router_b,
              moe_w_gate, moe_w_up, moe_w_down, final_norm_g):
    n = x.shape[1]
    ROWS = n // GRID_W
    rows = jnp.repeat(jnp.arange(ROWS), GRID_W)
    cols = jnp.tile(jnp.arange(GRID_W), ROWS)
    h, g = x, ctx
    for l in range(DEPTH):
        need_ctx = l < DEPTH - 1
        lam_init = 0.8 - 0.6 * math.exp(-0.3 * l)
        lp = {
            'w_in': w_in[l], 'pool_w': pool_w[l], 'pool_scale': pool_scale[l], 'pool_out': pool_out[l],
            'mla_kv_norm_g': mla_kv_norm_g[l], 'mla_w_kv_b': mla_w_kv_b[l], 'mla_out': mla_out[l],
            'win_sink': win_sink[l], 'win_out': win_out[l], 'diff_lambda': diff_lambda[l],
            'diff_subln_g': diff_subln_g[l], 'diff_out': diff_out[l], 'w_out': w_out[l],
        }
        mod = jax.nn.silu(c) @ w_mod[l] + b_mod[l]
        mod_c = jax.nn.silu(c_ctx) @ w_mod[l] + b_mod[l]
        sh1, sc1, gt1, sh2, sc2, gt2 = jnp.split(mod[:, None, :], 6, axis=-1)
        csh1, csc1, cgt1, csh2, csc2, cgt2 = jnp.split(mod_c, 6, axis=-1)
        u = _modulate(h, norm1_g[l], sh1, sc1)
        uc = _modulate(g, norm1_g[l], csh1, csc1)
        y, yc = _mixer_sublayer(u, uc, (rows, cols), lp, lam_init, need_ctx)
        h = h + gt1 * y
        j = l // 2
        if l % 2 == 0:
            ffn = lambda t, j=j: _swiglu(t, ffn_w_gate[j], ffn_w_up[j], ffn_w_down[j])
        else:
            ffn = lambda t, j=j: _moe(t, moe_router[j], moe_router_b[j], moe_w_gate[j], moe_w_up[j], moe_w_down[j])
        h = h + gt2 * ffn(_modulate(h, norm2_g[l], sh2, sc2))
        if need_ctx:
            g = g + cgt1 * yc
            g = g + cgt2 * ffn(_modulate(g, norm2_g[l], csh2, csc2))
    return _rmsnorm(h, final_norm_g)
```

```python
import contextlib
import math

import numpy as np
import ml_dtypes

import concourse.bass as bass
import concourse.mybir as mybir
from concourse.bass_utils import run_bass_kernel_spmd

F32 = mybir.dt.float32
BF16 = mybir.dt.bfloat16
AF = mybir.ActivationFunctionType
ALU = mybir.AluOpType
AX = mybir.AxisListType
NPBF = ml_dtypes.bfloat16

D = 2048
KT = 16
NL = 4096
NCX = 256
TT = 2 * NL + NCX
T = TT
TKEYS = TT
EPS = 1e-6
NQT = 28
NKT = 19
VW = 2304
GATE0 = 7744
N_E = 8
DFF = 7168

BLOCKS_ALL = [(i * 512, 512, 0) for i in range(16)] + [(2 * NL, 256, 1)]
BLOCKS = BLOCKS_ALL


class TK:
    __slots__ = ("w", "r", "key")

    def __init__(self):
        self.w = None
        self.r = {}
        self.key = None


class Ctx:
    def __init__(self):
        self.nc = bass.Bass("TRN2", target_bir_lowering=False)
        self.es = contextlib.ExitStack()
        nc = self.nc
        self.eng = dict(pe=nc.tensor, act=nc.scalar, dve=nc.vector, pool=nc.gpsimd, sp=nc.sync)
        self.sems = {}
        self.cnt = {}
        for e in ("pe", "act", "dve", "pool"):
            self.sems[e] = self.es.enter_context(nc.semaphore("s_" + e))
            self.cnt[e] = 0
        self.ebase = {e: 0 for e in ("pe", "act", "dve", "pool")}
        self.waited = {e: {} for e in self.eng}
        self.nid = 0
        self.finals = []
        self.nps = 0
        self.nsb = 0
        self.epoch = 0
        self.depth = 0
        self.scope_keys = []
        self.free_keys = []
        self.nsem = 0
        self.es_top = self.es
        self.bar_sem = self.es.enter_context(nc.semaphore("s_bar"))
        self.bar_cnt = 0
        self.bar_dram = nc.dram_tensor("bar_dram", [1, 16], F32, kind="Internal").ap()
        self.bar_sb = self.es.enter_context(nc.sbuf_tensor("sb_bar", [1, 16], F32))

    @contextlib.contextmanager
    def scope(self):
        old = self.es
        self.es = contextlib.ExitStack()
        self.depth += 1
        self.scope_keys.append([])
        try:
            yield
        finally:
            self.barrier()
            self.es.close()
            self.es = old
            self.depth -= 1
            keys = self.scope_keys.pop()
            if self.depth == 0:
                for k in keys:
                    if self.cnt[k] < 30000:
                        self.free_keys.append(k)
                self.epoch += 1
                for e in ("pe", "act", "dve", "pool"):
                    self.sems[e] = self.es_top.enter_context(self.nc.semaphore("s_%s_%d" % (e, self.epoch)))
                    self.cnt[e] = 0
                    for w in self.waited.values():
                        w.pop(e, None)
            else:
                self.scope_keys[-1].extend(keys)

    def barrier(self):
        for key, c in list(self.cnt.items()):
            if c > 0:
                self.wait("sp", key, c)
        self.bar_cnt += 16
        self.nc.sync.dma_start(out=self.bar_sb[:], in_=self.bar_dram).then_inc(self.bar_sem, 16)
        for e in ("pe", "act", "dve", "pool", "sp"):
            self.eng[e].wait_ge(self.bar_sem, self.bar_cnt)

    def sbuf(self, name, shape, dt):
        self.nsb += 1
        return self.es.enter_context(self.nc.sbuf_tensor("sb%d_%s" % (self.nsb, name), list(shape), dt))

    def psum(self, name=None):
        self.nps += 1
        return self.es.enter_context(self.nc.psum_tensor("ps%d" % self.nps, [128, 512], F32))

    def dram(self, name, shape, dt, kind):
        return self.nc.dram_tensor(name, list(shape), dt, kind=kind).ap()

    def sem_for(self, tk):
        if tk.key is None:
            if self.free_keys:
                tk.key = self.free_keys.pop()
            else:
                tk.key = ("d", self.nid)
                self.nid += 1
                self.sems[tk.key] = self.es_top.enter_context(self.nc.semaphore("d%d" % self.nid))
                self.cnt[tk.key] = 0
            if self.scope_keys:
                self.scope_keys[-1].append(tk.key)
        return tk.key

    def wait(self, e, key, val):
        if isinstance(key, tuple) and key[0] == "e":
            if key[2] != self.epoch:
                return
            key = key[1]
        if self.waited[e].get(key, 0) >= val:
            return
        self.waited[e][key] = val
        self.eng[e].wait_ge(self.sems[key], val)

    def deps(self, e, reads, writes):
        d = {}

        def add(ev):
            if ev is None:
                return
            k, v = ev
            if d.get(k, 0) < v:
                d[k] = v

        for t in reads:
            add(t.w)
        for t in writes:
            add(t.w)
            for k, v in t.r.items():
                if not (isinstance(k, tuple) and k[0] == "e" and k[1] == e):
                    add((k, v))
        for k, v in d.items():
            if isinstance(k, tuple) and k[0] == "e" and k[1] == e and e == "pe":
                continue
            self.wait(e, k, v)

    def done(self, ev, reads, writes):
        k, v = ev
        for t in reads:
            if t.r.get(k, 0) < v:
                t.r[k] = v
        for t in writes:
            t.w = ev
            t.r = {}

    def op(self, e, fn, reads=(), writes=()):
        self.deps(e, reads, writes)
        ins = fn(self.eng[e])
        self.cnt[e] += 1
        ins.then_inc(self.sems[e], 1)
        self.done((("e", e, self.epoch), self.cnt[e]), reads, writes)
        return ins

    def mm(self, out, pairs, reads=(), writes=()):
        self.deps("pe", reads, writes)
        n = len(pairs)
        ins = None
        for i, (l, r) in enumerate(pairs):
            ins = self.nc.tensor.matmul(out, l, r, start=(i == 0), stop=(i == n - 1))
        self.cnt["pe"] += 1
        ins.then_inc(self.sems["pe"], 1)
        self.done((("e", "pe", self.epoch), self.cnt["pe"]), reads, writes)

    def pe_raw(self, emit, reads=(), writes=()):
        self.deps("pe", reads, writes)
        ins = emit(self.nc.tensor)
        self.cnt["pe"] += 1
        ins.then_inc(self.sems["pe"], 1)
        self.done((("e", "pe", self.epoch), self.cnt["pe"]), reads, writes)

    def dma(self, e, out, in_, reads=(), writes=(), final=False):
        self.deps(e, reads, writes)
        tk = writes[0] if writes else reads[0]
        key = self.sem_for(tk)
        ins = self.eng[e].dma_start(out=out, in_=in_)
        self.cnt[key] += 16
        ins.then_inc(self.sems[key], 16)
        self.done((key, self.cnt[key]), reads, writes)
        if final:
            self.finals.append((key, self.cnt[key]))

    def finish(self):
        for key, v in self.finals:
            self.wait("sp", key, v)
        for e in ("pe", "act", "dve", "pool"):
            if self.cnt[e] > 0:
                self.wait("sp", e, self.cnt[e])


class Ring:
    def __init__(self, cx, name, n, shape, dt):
        self.t = [cx.sbuf("%s%d" % (name, i), shape, dt) for i in range(n)]
        self.tk = [TK() for _ in range(n)]
        self.i = 0
        self.n = n

    def next(self):
        j = self.i % self.n
        self.i += 1
        return self.t[j], self.tk[j]


class PsRing:
    def __init__(self, cx, n):
        self.t = [cx.psum() for _ in range(n)]
        self.tk = [TK() for _ in range(n)]
        self.i = 0
        self.n = n

    def next(self):
        j = self.i % self.n
        self.i += 1
        return self.t[j], self.tk[j]


class WStream:
    def __init__(self, cx, items, nslots=4, slot=8192, eng="pool"):
        self.cx = cx
        self.items = items
        self.ring = Ring(cx, "wring", nslots, [128, slot], BF16)
        self.issued = []
        self.u = 0
        self.eng = eng

    def _issue(self):
        j = len(self.issued)
        src, a, b = self.items[j]
        t, tk = self.ring.next()
        view = t[:, 0:a * b].rearrange("p (a b) -> p a b", a=a)
        self.cx.dma(self.eng, view, src, writes=[tk])
        self.issued.append((view, tk))

    def get_many(self, m):
        assert m <= self.ring.n
        while len(self.issued) < min(len(self.items), self.u + self.ring.n):
            self._issue()
        r = self.issued[self.u:self.u + m]
        self.u += m
        return r

    def get(self):
        return self.get_many(1)[0]


def wsrc(w2d, r0, nk, c0, nc_):
    return w2d[r0:r0 + nk * 128, c0:c0 + nc_].rearrange("(k p) c -> p k c", p=128)


def load_const(cx, name, src, shape, dt=F32, eng="sp"):
    t = cx.sbuf(name, shape, dt)
    tk = TK()
    cx.dma(eng, t[:], src, writes=[tk])
    return t, tk


def mod_feature_major(cx, ws, silu_t, silu_tk, bmod_t, bmod_tk, col_tiles, out_t, out_tk, psr):
    for g0 in range(0, len(col_tiles), 4):
        w, wtk = ws.get()
        for jj in range(4):
            j = g0 + jj
            ct = col_tiles[j]
            ps, ptk = psr.next()
            cx.mm(ps[:, 0:2], [(w[:, k, jj * 128:(jj + 1) * 128], silu_t[:, k, :]) for k in range(KT)],
                  reads=[wtk, silu_tk], writes=[ptk])
            cx.op("dve", lambda e, ps=ps, j=j, ct=ct: e.tensor_tensor(
                out_t[:, j, :], ps[:, 0:2], bmod_t[:, ct, :], ALU.add),
                reads=[ptk, bmod_tk], writes=[out_tk])


def bmod2(bmod_t, ct):
    return bmod_t[:, ct, :]


def mod_rows(cx, ws, silurep_t, silurep_tk, brow_t, brow_tk, ncol, out_t, out_tk, psr, variant):
    for g in range(ncol // 512):
        w, wtk = ws.get()
        ps, ptk = psr.next()
        cx.mm(ps[:, :], [(silurep_t[:, variant, k, :], w[:, k, :]) for k in range(KT)],
              reads=[wtk, silurep_tk], writes=[ptk])
        cx.op("dve", lambda e, ps=ps, g=g: e.tensor_tensor(out_t[:, g * 512:(g + 1) * 512], ps[:, :],
                                                           brow_t[:, g * 512:(g + 1) * 512], ALU.add),
              reads=[ptk, brow_tk], writes=[out_tk])


def prep_silu(cx, cfm_ap, need_rep):
    c_t, c_tk = load_const(cx, "cfm", cfm_ap, [128, KT, 2])
    s_t = cx.sbuf("silu_c", [128, KT, 2], BF16)
    s_tk = TK()
    cx.op("act", lambda e: e.activation(out=s_t[:], in_=c_t[:], func=AF.Silu), reads=[c_tk], writes=[s_tk])
    rep_t = rep_tk = None
    if need_rep:
        s32 = cx.sbuf("silu_c32", [128, KT, 2], F32)
        s32_tk = TK()
        cx.op("act", lambda e: e.activation(out=s32[:], in_=c_t[:], func=AF.Silu), reads=[c_tk], writes=[s32_tk])
        ones = cx.sbuf("ones_rep", [128, 128], F32)
        o_tk = TK()
        cx.op("dve", lambda e: e.memset(ones[:], 1.0), writes=[o_tk])
        rep_t = cx.sbuf("silu_rep", [128, 2, KT, 128], BF16)
        rep_tk = TK()
        for v in range(2):
            for k in range(KT):
                cx.op("act", lambda e, v=v, k=k: e.activation(out=rep_t[:, v, k, :], in_=ones[:], func=AF.Copy,
                                                              scale=s32[:, k, v:v + 1]),
                      reads=[s32_tk, o_tk], writes=[rep_tk])
    return s_t, s_tk, rep_t, rep_tk


class NormT:
    def __init__(self, cx, ident_t, ident_tk, A_t, A_tk, B_t, B_tk, psr):
        self.cx = cx
        self.hr = Ring(cx, "nt_h", 2, [128, D], F32)
        self.un = Ring(cx, "nt_un", 2, [128, D], F32)
        self.junk = cx.sbuf("nt_junk", [128, D], BF16)
        self.junk_tk = TK()
        self.st = Ring(cx, "nt_st", 4, [128, 4], F32)
        self.ident_t, self.ident_tk = ident_t, ident_tk
        self.A_t, self.A_tk, self.B_t, self.B_tk = A_t, A_tk, B_t, B_tk
        self.psr = psr
        self.flip = 0

    def run(self, h_ap, N, variant, uT, uT_tk, post=None):
        cx = self.cx
        for s in range(N // 128):
            ht, htk = self.hr.next()
            cx.dma("sp", ht[:], h_ap[s * 128:(s + 1) * 128, :], writes=[htk])
            st, stk = self.st.next()
            cx.op("act", lambda e: e.activation(out=self.junk[:], in_=ht[:], func=AF.Square, accum_out=st[:, 0:1]),
                  reads=[htk], writes=[self.junk_tk, stk])
            cx.op("act", lambda e: e.activation(out=st[:, 1:2], in_=st[:, 0:1], func=AF.Sqrt, scale=1.0 / D, bias=EPS_T[0][:, 0:1]),
                  reads=[stk, EPS_T[1]], writes=[stk])
            cx.op("dve", lambda e: e.reciprocal(st[:, 2:3], st[:, 1:2]), reads=[stk], writes=[stk])
            un, untk = self.un.next()
            cx.op("act", lambda e: e.activation(out=un[:], in_=ht[:], func=AF.Copy, scale=st[:, 2:3]),
                  reads=[htk, stk], writes=[untk])
            for kg in range(4):
                ps, ptk = self.psr.next()

                def emit(pe, ps=ps, kg=kg, un=un):
                    ins = None
                    for kk in range(4):
                        k = kg * 4 + kk
                        ins = pe.transpose(ps[:, kk * 128:(kk + 1) * 128], un[:, k * 128:(k + 1) * 128], self.ident_t[:])
                    return ins

                cx.pe_raw(emit, reads=[untk, self.ident_tk], writes=[ptk])
                for kk in range(4):
                    k = kg * 4 + kk
                    self.flip ^= 1
                    if self.flip:
                        cx.op("act", lambda e, k=k, kk=kk, ps=ps: e.activation(
                            out=uT[:, k, s * 128:(s + 1) * 128], in_=ps[:, kk * 128:(kk + 1) * 128], func=AF.Identity,
                            scale=self.A_t[:, k, variant:variant + 1], bias=self.B_t[:, k, variant:variant + 1]),
                            reads=[ptk, self.A_tk, self.B_tk], writes=[uT_tk])
                    else:
                        cx.op("dve", lambda e, k=k, kk=kk, ps=ps: e.tensor_scalar(
                            uT[:, k, s * 128:(s + 1) * 128], ps[:, kk * 128:(kk + 1) * 128],
                            self.A_t[:, k, variant:variant + 1], self.B_t[:, k, variant:variant + 1], ALU.mult, ALU.add),
                            reads=[ptk, self.A_tk, self.B_tk], writes=[uT_tk])


EPS_T = [None, None]


def make_eps(cx, val=EPS):
    t = cx.sbuf("eps_c", [128, 1], F32)
    tk = TK()
    cx.op("dve", lambda e: e.memset(t[:], val), writes=[tk])
    EPS_T[0], EPS_T[1] = t, tk


def make_ident(cx, ident_ap):
    return load_const(cx, "ident", ident_ap, [128, 128])


def make_AB(cx, modT, modT_tk, j_sc, j_sh, gfm_t, gfm_tk, name):
    A = cx.sbuf(name + "_A", [128, KT, 2], F32)
    B = cx.sbuf(name + "_B", [128, KT, 2], F32)
    atk, btk = TK(), TK()
    cx.op("dve", lambda e: e.tensor_scalar(A[:], modT[:, j_sc:j_sc + KT, :], 1.0, None, ALU.add),
          reads=[modT_tk], writes=[atk])
    for v in range(2):
        cx.op("dve", lambda e, v=v: e.tensor_tensor(A[:, :, v], A[:, :, v], gfm_t[:, :], ALU.mult),
              reads=[gfm_tk, atk], writes=[atk])
    cx.op("dve", lambda e: e.tensor_copy(B[:], modT[:, j_sh:j_sh + KT, :]), reads=[modT_tk], writes=[btk])
    return A, atk, B, btk


ROPE_TILES = (
    [("q", 8 + j, 0) for j in range(4)] + [("k", 8, 0)]
    + [("q", 12 + h, 1) for h in range(8)] + [("k", 9 + h, 1) for h in range(2)]
    + [("q", 20 + j, 1) for j in range(8)] + [("k", 11 + j, 1) for j in range(8)]
)
W1_COLS = 5 * 512 + len(ROPE_TILES) * 256 + 1280


def w1_col_index():
    idx = []
    idx += list(range(0, 1024))
    for h in range(8):
        idx += list(range(1024 + 192 * h, 1024 + 192 * h + 128))
    idx += list(range(2560, 3072))

    def perm(cols, d):
        q = d // 4
        out = []
        for b0 in range(0, len(cols), d):
            blk = cols[b0:b0 + d]
            out += blk[q:2 * q] + blk[0:q] + blk[3 * q:4 * q] + blk[2 * q:3 * q]
        return out

    ropes = []
    for j in range(4):
        c = []
        for h in (2 * j, 2 * j + 1):
            c += list(range(1024 + 192 * h + 128, 1024 + 192 * h + 192))
        ropes.append((c, 64))
    ropes.append((list(range(3072, 3136)) * 2, 64))
    for h in range(8):
        ropes.append((list(range(3136 + 128 * h, 3136 + 128 * h + 128)), 128))
    for h in range(2):
        ropes.append((list(range(4160 + 128 * h, 4160 + 128 * h + 128)), 128))
    for j in range(8):
        ropes.append((list(range(4672 + 128 * j, 4672 + 128 * j + 128)), 128))
    for j in range(8):
        ropes.append((list(range(5696 + 128 * j, 5696 + 128 * j + 128)), 128))
    assert len(ropes) == len(ROPE_TILES)
    for c, d in ropes:
        idx += c + perm(c, d)
    idx += list(range(4416, 4672))
    idx += list(range(6720, 7744))
    assert len(idx) == W1_COLS
    return np.asarray(idx, dtype=np.int64)


def kvb_col_index():
    idx = []
    for h in range(8):
        idx += list(range(256 * h, 256 * h + 128))
    for h in range(8):
        idx += list(range(256 * h + 128, 256 * h + 256))
    return np.asarray(idx, dtype=np.int64)


def emit_p1(cx, l, G):
    h_in = G("h_in")
    w1 = G("w1")
    wkvb = G("wkvb")
    wmod = G("wmod")
    bmodfm = G("bmodfm")
    cfm = G("cfm")
    g1fm = G("g1fm")
    kvgfm = G("kvgfm")
    ident = G("ident")
    rope = G("rope")
    UT = G("UT")
    QT = G("QT")
    KTo = G("KT")
    Vo = G("V")
    AT = G("AT")

    make_eps(cx)
    ident_t, ident_tk = make_ident(cx, ident)
    psr = PsRing(cx, 8)

    items = []
    for g in range(8):
        items.append((wsrc(wmod, 0, KT, g * 512, 512), KT, 512))
    for (tok0, N, var) in BLOCKS:
        c0 = 0
        for g in range(5):
            items.append((wsrc(w1, 0, KT, c0, 512), KT, 512))
            c0 += 512
        items.append((wsrc(wkvb, 0, 4, 0, 2048), 4, 2048))
        nr = len(ROPE_TILES)
        for g in range((nr + 1) // 2):
            ncol = 512 if 2 * g + 1 < nr else 256
            items.append((wsrc(w1, 0, KT, c0, ncol), KT, ncol))
            c0 += ncol
        for ncol in (512, 512, 256):
            items.append((wsrc(w1, 0, KT, c0, ncol), KT, ncol))
            c0 += ncol
        assert c0 == W1_COLS
    ws = WStream(cx, items)

    s_t, s_tk, _, _ = prep_silu(cx, cfm, False)
    bm_t, bm_tk = load_const(cx, "bmodfm", bmodfm, [128, 32, 2])
    g1_t, g1_tk = load_const(cx, "g1fm", g1fm, [128, KT])
    kvg_t, kvg_tk = load_const(cx, "kvgfm", kvgfm, [128, 4])
    modT = cx.sbuf("modT", [128, 32, 2], F32)
    modT_tk = TK()
    mod_feature_major(cx, ws, s_t, s_tk, bm_t, bm_tk, list(range(32)), modT, modT_tk, psr)
    A_t, A_tk, B_t, B_tk = make_AB(cx, modT, modT_tk, 16, 0, g1_t, g1_tk, "m1")
    nt = NormT(cx, ident_t, ident_tk, A_t, A_tk, B_t, B_tk, psr)

    ones32 = cx.sbuf("ones32", [128, 128], F32)
    ones_tk = TK()
    cx.op("dve", lambda e: e.memset(ones32[:], 1.0), writes=[ones_tk])

    uTr = Ring(cx, "uT", 2, [128, KT, 512], BF16)
    stg = Ring(cx, "stg", 6, [128, 512], BF16)
    vstg = Ring(cx, "vstg", 3, [128, 1024], BF16)
    ropeR = Ring(cx, "ropeR", 2, [128, 4, 512], F32)
    t1r = Ring(cx, "t1r", 2, [128, 512], F32)
    t2r = Ring(cx, "t2r", 2, [128, 512], F32)
    ckv_t = cx.sbuf("ckv", [128, 4, 512], F32)
    ckv_tk = TK()
    ckvsq = Ring(cx, "ckvsq", 2, [128, 512], F32)
    rs_t = cx.sbuf("ckv_rs", [128, 512], F32)
    rs_tk = TK()
    ckvn_t = cx.sbuf("ckvn", [128, 4, 512], BF16)
    ckvn_tk = TK()
    flip = [0]

    def evac_copy(ps, ptk, N, dst_ap):
        st, stk = stg.next()
        flip[0] ^= 1
        if flip[0]:
            cx.op("act", lambda e: e.activation(out=st[:, :N], in_=ps[:, :N], func=AF.Copy), reads=[ptk], writes=[stk])
        else:
            cx.op("dve", lambda e: e.tensor_copy(st[:, :N], ps[:, :N]), reads=[ptk], writes=[stk])
        cx.dma("sp", dst_ap, st[:, :N], reads=[stk])

    for (tok0, N, var) in BLOCKS:
        ts = slice(tok0, tok0 + N)
        uT, uT_tk = uTr.next()
        nt.run(h_in[tok0:tok0 + N, :], N, var, uT, uT_tk)
        cx.dma("sp", UT[:, :, ts].rearrange("k p t -> p k t"), uT[:, :, :N], reads=[uT_tk])
        rp, rptk = ropeR.next()
        cx.dma("sp", rp[:, :, :N], rope[:, :, ts].rearrange("c p t -> p c t"), writes=[rptk])

        def proj_tile(w, wtk, j):
            ps, ptk = psr.next()
            cx.mm(ps[:, :N], [(w[:, k, j * 128:(j + 1) * 128], uT[:, k, :N]) for k in range(KT)],
                  reads=[wtk, uT_tk], writes=[ptk])
            return ps, ptk

        for g in range(4):
            w, wtk = ws.get()
            for j in range(4):
                ps, ptk = proj_tile(w, wtk, j)
                tile_i = (g % 2) * 4 + j
                dst = AT[tile_i, :, ts] if g < 2 else QT[tile_i, :, ts]
                evac_copy(ps, ptk, N, dst)
        w, wtk = ws.get()
        for j in range(4):
            ps, ptk = proj_tile(w, wtk, j)
            cx.op("act", lambda e, ps=ps, j=j: e.activation(out=ckv_t[:, j, :N], in_=ps[:, :N], func=AF.Copy),
                  reads=[ptk], writes=[ckv_tk])
        pss, pstk = psr.next()
        sqs = []
        for j in range(4):
            sq, sqtk = ckvsq.next()
            cx.op("act", lambda e, sq=sq, j=j: e.activation(out=sq[:, :N], in_=ckv_t[:, j, :N], func=AF.Square),
                  reads=[ckv_tk], writes=[sqtk])
            cx.pe_raw(lambda pe, sq=sq, j=j: pe.matmul(pss[:, :N], ones32[:], sq[:, :N], start=(j == 0), stop=(j == 3)),
                      reads=[sqtk, ones_tk], writes=[pstk])
        cx.op("act", lambda e: e.activation(out=rs_t[:, :N], in_=pss[:, :N], func=AF.Sqrt, scale=1.0 / 512, bias=EPS_T[0][:, 0:1]),
              reads=[pstk, EPS_T[1]], writes=[rs_tk])
        cx.op("dve", lambda e: e.reciprocal(rs_t[:, :N], rs_t[:, :N]), reads=[rs_tk], writes=[rs_tk])
        for j in range(4):
            cx.op("dve", lambda e, j=j: e.scalar_tensor_tensor(ckvn_t[:, j, :N], ckv_t[:, j, :N], kvg_t[:, j:j + 1],
                                                               rs_t[:, :N], ALU.mult, ALU.mult),
                  reads=[ckv_tk, rs_tk, kvg_tk], writes=[ckvn_tk])
        w, wtk = ws.get()
        for h in range(8):
            ps, ptk = psr.next()
            cx.mm(ps[:, :N], [(w[:, k, h * 128:(h + 1) * 128], ckvn_t[:, k, :N]) for k in range(4)],
                  reads=[wtk, ckvn_tk], writes=[ptk])
            evac_copy(ps, ptk, N, KTo[h, :, ts])
        for s in range(N // 128):
            vs, vstk = vstg.next()
            for c in range(2):
                ps, ptk = psr.next()
                cx.mm(ps[:, :], [(ckvn_t[:, k, s * 128:(s + 1) * 128], w[:, k, 1024 + c * 512:1024 + (c + 1) * 512]) for k in range(4)],
                      reads=[wtk, ckvn_tk], writes=[ptk])
                cx.op("act", lambda e, ps=ps, c=c, vs=vs: e.activation(out=vs[:, c * 512:(c + 1) * 512], in_=ps[:, :], func=AF.Copy),
                      reads=[ptk], writes=[vstk])
            cx.dma("sp", Vo[tok0 + s * 128:tok0 + (s + 1) * 128, 0:1024], vs[:, :], reads=[vstk])
        nr = len(ROPE_TILES)
        for g in range((nr + 1) // 2):
            w, wtk = ws.get()
            for jj in range(2):
                ri = 2 * g + jj
                if ri >= nr:
                    break
                dest, dtile, tabt = ROPE_TILES[ri]
                psx, ptkx = proj_tile(w, wtk, 2 * jj)
                psp, ptkp = proj_tile(w, wtk, 2 * jj + 1)
                t1, t1k = t1r.next()
                t2, t2k = t2r.next()
                cx.op("dve", lambda e, psx=psx, t1=t1, tabt=tabt: e.tensor_tensor(t1[:, :N], psx[:, :N], rp[:, 2 * tabt, :N], ALU.mult),
                      reads=[ptkx, rptk], writes=[t1k])
                cx.op("dve", lambda e, psp=psp, t2=t2, tabt=tabt: e.tensor_tensor(t2[:, :N], psp[:, :N], rp[:, 2 * tabt + 1, :N], ALU.mult),
                      reads=[ptkp, rptk], writes=[t2k])
                st, stk = stg.next()
                cx.op("pool", lambda e, st=st, t1=t1, t2=t2: e.tensor_tensor(st[:, :N], t1[:, :N], t2[:, :N], ALU.add),
                      reads=[t1k, t2k], writes=[stk])
                dst = QT[dtile, :, ts] if dest == "q" else KTo[dtile, :, ts]
                cx.dma("sp", dst, st[:, :N], reads=[stk])
        wl = ws.get_many(3)
        for s in range(N // 128):
            vs, vstk = vstg.next()
            vs2, vstk2 = vstg.next()
            for gi, (w, wtk) in enumerate(wl):
                ncol = 512 if gi < 2 else 256
                ps, ptk = psr.next()
                cx.mm(ps[:, :ncol], [(uT[:, k, s * 128:(s + 1) * 128], w[:, k, :ncol]) for k in range(KT)],
                      reads=[wtk, uT_tk], writes=[ptk])
                if gi == 0:
                    cx.op("act", lambda e, ps=ps, vs=vs: e.activation(out=vs[:, 0:512], in_=ps[:, :], func=AF.Copy),
                          reads=[ptk], writes=[vstk])
                elif gi == 1:
                    cx.op("dve", lambda e, ps=ps, vs=vs: e.tensor_copy(vs[:, 512:1024], ps[:, :]), reads=[ptk], writes=[vstk])
                else:
                    cx.op("act", lambda e, ps=ps, vs2=vs2: e.activation(out=vs2[:, 0:256], in_=ps[:, :256], func=AF.Copy),
                          reads=[ptk], writes=[vstk2])
            r0 = tok0 + s * 128
            cx.dma("sp", Vo[r0:r0 + 128, 1024:2048], vs[:, :], reads=[vstk])
            cx.dma("sp", Vo[r0:r0 + 128, 2048:2304], vs2[:, 0:256], reads=[vstk2])


def fm(v):
    return np.ascontiguousarray(v.reshape(-1, 128).T)


_W1_IDX = w1_col_index()
_KVB_IDX = kvb_col_index()
IDENT = np.eye(128, dtype=np.float32)


HL = 128 + NL + 128
PL = 8 + NL + 8
PLC = 8 + NCX + 8


def emit_p2(cx, l, G):
    need_ctx = (l == 0)
    lam_init = 0.8 - 0.6 * math.exp(-0.3 * l)
    QT = G("QT")
    KA = G("KT")
    VA = G("V")
    wmask = G("wmask")
    AT = G("AT")
    pflag = G("pflag")
    invcnt = G("invcnt")
    poolw = G("poolw")
    pscfm = G("pscfm")
    dlam = G("dlam")
    dgfm = G("dgfm")
    sink = G("sink")
    BT = G("BT")

    make_eps(cx)
    psS = PsRing(cx, 2)
    psA = [PsRing(cx, 3), PsRing(cx, 3)]
    ones_bf = cx.sbuf("ones_bf", [128, 128], BF16)
    ones_bf_tk = TK()
    cx.op("dve", lambda e: e.memset(ones_bf[:], 1.0), writes=[ones_bf_tk])
    ones32 = cx.sbuf("ones32", [128, 128], F32)
    ones_tk = TK()
    cx.op("dve", lambda e: e.memset(ones32[:], 1.0), writes=[ones_tk])
    pTr = Ring(cx, "pT", 3, [128, 512], BF16)
    outst = Ring(cx, "outst", 4, [128, 512], BF16)
    rcp = Ring(cx, "rcp", 2, [128, 512], F32)
    setflip = [0]

    qblocks = [(i * 512, 512) for i in range(16 if need_ctx else 8)]
    cblock = (2 * NL, 256)

    sacc_r = Ring(cx, "sacc", 2, [128, 512], F32)
    LOOK = 2

    def attn(N, q_parts, q_tks, kt_list, scale, nO, finish, mask_of=None):
        aset = psA[setflip[0]]
        setflip[0] ^= 1
        accs = [aset.t[j] for j in range(3)]
        atk = aset.tk
        n = len(kt_list)
        sacc, sacc_tk = sacc_r.next()
        stages = []

        def issue_S(i):
            kparts, vparts, tks, mask = kt_list[i]
            ps, ptk = psS.next()
            cx.mm(ps[:, :N], list(zip(kparts, q_parts)), reads=list(tks) + list(q_tks), writes=[ptk])
            pt, pttk = pTr.next()
            cx.op("act", lambda e, ps=ps, pt=pt: e.activation(out=pt[:, :N], in_=ps[:, :N], func=AF.Exp, scale=scale),
                  reads=[ptk], writes=[pttk])
            if mask is not None:
                mk_ap, mk_tk = mask
                cx.op("pool", lambda e, pt=pt, mk_ap=mk_ap: e.tensor_tensor(pt[:, :N], pt[:, :N], mk_ap[:, :N], ALU.mult),
                      reads=[mk_tk, pttk], writes=[pttk])
            if i == 0:
                cx.op("dve", lambda e, pt=pt: e.tensor_copy(sacc[:, :N], pt[:, :N]), reads=[pttk], writes=[sacc_tk])
            else:
                cx.op("dve", lambda e, pt=pt: e.tensor_tensor(sacc[:, :N], sacc[:, :N], pt[:, :N], ALU.add),
                      reads=[pttk, sacc_tk], writes=[sacc_tk])
            stages.append((pt, pttk, vparts, tks))

        def issue_PV(i):
            pt, pttk, vparts, tks = stages[i]

            def emit(pe, pt=pt, vparts=vparts, i=i):
                ins = None
                for j in range(nO):
                    ins = pe.matmul(accs[j][:, :N], vparts[j], pt[:, :N], start=(i == 0), stop=(i == n - 1))
                return ins

            cx.pe_raw(emit, reads=[pttk] + list(tks), writes=[atk[j] for j in range(nO)])

        for i in range(n + LOOK):
            if i < n:
                issue_S(i)
            if i - LOOK >= 0:
                issue_PV(i - LOOK)
        cx.pe_raw(lambda pe: pe.matmul(accs[2][:, :N], ones32[:], sacc[:, :N], start=True, stop=True),
                  reads=[sacc_tk, ones_tk], writes=[atk[2]])
        finish(accs, atk, N)

    def load(ring, src, shape_view=None):
        t, tk = ring.next()
        dst = t[:] if shape_view is None else shape_view(t)
        cx.dma("sp", dst, src, writes=[tk])
        return t, tk

    with cx.scope():
        pw_t = cx.sbuf("poolw", [128, 8, 256], BF16)
        pw_tk = TK()
        cx.dma("pool", pw_t[:], poolw.rearrange("g (k p) d -> p (g k) d", p=128), writes=[pw_tk])
        psc_t, psc_tk = load_const(cx, "pscfm", pscfm, [128, 8])
        inv_t, inv_tk = load_const(cx, "invcnt", invcnt, [128, 4, 48])
        pf_t, pf_tk = load_const(cx, "pflag", pflag, [128, 4])
        xr = Ring(cx, "pool_x", 2, [128, PL], BF16)
        sA = cx.sbuf("pool_sA", [128, PL], F32)
        sB = cx.sbuf("pool_sB", [128, PL], F32)
        sA_tk, sB_tk = TK(), TK()
        pooled = [cx.sbuf("pooled%d" % i, [128, TT], BF16) for i in range(2)]
        pooled_tk = [TK(), TK()]
        for gi, w in enumerate((2, 4, 8, 16)):
            for seg in ((0, 1, 2) if need_ctx else (0,)):
                n_tok = NCX if seg == 2 else NL
                L = n_tok + 16
                o0 = seg * NL
                oth = (1 - seg) * NL
                for k in range(2):
                    x, xtk = xr.next()
                    src = AT[2 * gi + k, :, :]
                    cx.dma("sp", x[:, 8:8 + n_tok], src[:, o0:o0 + n_tok], writes=[xtk])
                    if seg == 2:
                        cx.op("dve", lambda e, x=x: e.memset(x[:, 0:8], 0.0), writes=[xtk])
                        cx.op("dve", lambda e, x=x, n_tok=n_tok: e.memset(x[:, 8 + n_tok:16 + n_tok], 0.0), writes=[xtk])
                    else:
                        cx.dma("sp", x[:, 0:8], src[:, oth + NL - 8:oth + NL], writes=[xtk])
                        cx.dma("sp", x[:, 8 + n_tok:16 + n_tok], src[:, oth:oth + 8], writes=[xtk])
                        cx.op("dve", lambda e, x=x, seg=seg: e.tensor_scalar(x[:, 0:8], x[:, 0:8], pf_t[:, 2 * seg:2 * seg + 1], None, ALU.mult),
                              reads=[pf_tk], writes=[xtk])
                        cx.op("dve", lambda e, x=x, seg=seg, n_tok=n_tok: e.tensor_scalar(
                            x[:, 8 + n_tok:16 + n_tok], x[:, 8 + n_tok:16 + n_tok], pf_t[:, 2 * seg + 1:2 * seg + 2], None, ALU.mult),
                              reads=[pf_tk], writes=[xtk])
                    cur, cur_tk, ln = x, xtk, L
                    step = 1
                    bufs = [(sA, sA_tk), (sB, sB_tk)]
                    bi = 0
                    while step < w:
                        dst, dst_tk = bufs[bi]
                        bi ^= 1
                        ln2 = ln - step
                        cx.op("dve", lambda e, dst=dst, cur=cur, ln2=ln2, step=step: e.tensor_tensor(
                            dst[:, :ln2], cur[:, :ln2], cur[:, step:step + ln2], ALU.add), reads=[cur_tk], writes=[dst_tk])
                        cur, cur_tk, ln = dst, dst_tk, ln2
                        step *= 2
                    off = 8 - w // 2
                    pk = pooled[k]
                    cx.op("dve", lambda e, cur=cur, x=x, pk=pk, off=off, n_tok=n_tok, o0=o0: e.scalar_tensor_tensor(
                        pk[:, o0:o0 + n_tok], cur[:, off:off + n_tok], 1.0 / w, x[:, 8:8 + n_tok], ALU.mult, ALU.subtract),
                        reads=[cur_tk, xtk], writes=[pooled_tk[k]])
                    for ei, (a0, c0) in enumerate(((0, seg * 16), (n_tok - 8, seg * 16 + 8))):
                        tmp, tmptk = rcp.next()
                        cx.op("dve", lambda e, cur=cur, tmp=tmp, a0=a0, c0=c0, off=off: e.tensor_tensor(
                            tmp[:, 0:8], cur[:, off + a0:off + a0 + 8], inv_t[:, gi, c0:c0 + 8], ALU.mult),
                            reads=[cur_tk, inv_tk], writes=[tmptk])
                        cx.op("dve", lambda e, tmp=tmp, x=x, pk=pk, a0=a0, o0=o0: e.tensor_tensor(
                            pk[:, o0 + a0:o0 + a0 + 8], tmp[:, 0:8], x[:, 8 + a0:8 + a0 + 8], ALU.subtract),
                            reads=[tmptk, xtk, pooled_tk[k]], writes=[pooled_tk[k]])
            for (tok0, N) in (qblocks + ([cblock] if need_ctx else [])):
                for j in range(2):
                    ps, ptk = psS.next()
                    cx.mm(ps[:, :N], [(pw_t[:, 2 * gi + k, j * 128:(j + 1) * 128], pooled[k][:, tok0:tok0 + N]) for k in range(2)],
                          reads=[pw_tk] + pooled_tk, writes=[ptk])
                    st, stk = outst.next()
                    cx.op("act", lambda e, ps=ps, st=st, j=j: e.activation(out=st[:, :N], in_=ps[:, :N], func=AF.Copy,
                                                                           scale=psc_t[:, 2 * gi + j:2 * gi + j + 1]),
                          reads=[ptk, psc_tk], writes=[stk])
                    cx.dma("sp", BT[2 * gi + j, :, tok0:tok0 + N], st[:, :N], reads=[stk])


    def simple_finish(dst_tile):
        def fin(accs, atk, N, tok0):
            r, rtk = rcp.next()
            cx.op("dve", lambda e: e.reciprocal(r[:, :N], accs[2][:, :N]), reads=[atk[2]], writes=[rtk])
            st, stk = outst.next()
            cx.op("dve", lambda e: e.tensor_tensor(st[:, :N], accs[0][:, :N], r[:, :N], ALU.mult),
                  reads=[atk[0], rtk], writes=[stk])
            cx.dma("sp", BT[dst_tile, :, tok0:tok0 + N], st[:, :N], reads=[stk])
        return fin

    with cx.scope():
        MLA_SCALE = 192.0 ** -0.5
        kr = Ring(cx, "att_k", 2, [128, TKEYS], BF16)
        vr = Ring(cx, "att_v", 2, [128, 66, 128], BF16)
        qr = Ring(cx, "att_q", 2, [128, T], BF16)
        krope_t, krope_tk = load_const(cx, "krope", KA[8, :, :], [128, TKEYS], BF16)
        qrope = Ring(cx, "att_qr", 2, [128, T], BF16)
        for h in range(8):
            k_t, k_tk = load(kr, KA[h, :, :])
            v_t, v_tk = vr.next()
            cx.dma("sp", v_t[:, :, 0:128], VA[:, h * 128:(h + 1) * 128].rearrange("(kt p) c -> p kt c", p=128), writes=[v_tk])
            q_t, q_tk = load(qr, QT[h, :, :])
            q2_t, q2_tk = load(qrope, QT[8 + h // 2, :, :])
            pb = 64 * (h % 2)
            fin = simple_finish(8 + h)
            for (tok0, N) in (qblocks + ([cblock] if need_ctx else [])):
                is_c = tok0 >= 2 * NL
                tiles = range(64, 66) if is_c else range(66)
                kl = [([k_t[:, kt * 128:(kt + 1) * 128], krope_t[pb:pb + 64, kt * 128:(kt + 1) * 128]],
                       [v_t[:, kt, 0:128]], [k_tk, v_tk, krope_tk], None) for kt in tiles]
                attn(N, [q_t[:, tok0:tok0 + N], q2_t[pb:pb + 64, tok0:tok0 + N]], [q_tk, q2_tk], kl, MLA_SCALE, 1,
                     lambda accs, atk, N, tok0=tok0: fin(accs, atk, N, tok0))

    with cx.scope():
        WIN_SCALE = 128.0 ** -0.5
        kr = Ring(cx, "wc_k", 2, [128, TT], BF16)
        vr = Ring(cx, "wc_v", 2, [128, 66, 128], BF16)
        qr = Ring(cx, "wq", 2, [128, TT], BF16)
        mk_t, mk_tk = load_const(cx, "wmask", wmask.rearrange("m p q -> p m q"), [128, 10, 512], BF16)
        sk_t, sk_tk = load_const(cx, "sink", sink.partition_broadcast(128), [128, 8])
        cx.op("act", lambda e: e.activation(out=sk_t[:], in_=sk_t[:], func=AF.Exp), reads=[sk_tk], writes=[sk_tk])
        for kv in range(2):
            kw_t, kw_tk = load(kr, KA[9 + kv, :, :])
            vw_t, vw_tk = load(vr, VA[:, 1024 + kv * 128:1024 + (kv + 1) * 128].rearrange("(kt p) c -> p kt c", p=128))
            for gi in range(4):
                h = kv * 4 + gi
                q_t, q_tk = load(qr, QT[12 + h, :, :])

                def finw(accs, atk, N, tok0, h=h):
                    r, rtk = rcp.next()
                    cx.op("dve", lambda e: e.tensor_scalar(r[:, :N], accs[2][:, :N], sk_t[:, h:h + 1], None, ALU.add),
                          reads=[atk[2], sk_tk], writes=[rtk])
                    cx.op("dve", lambda e: e.reciprocal(r[:, :N], r[:, :N]), reads=[rtk], writes=[rtk])
                    st, stk = outst.next()
                    cx.op("dve", lambda e: e.tensor_tensor(st[:, :N], accs[0][:, :N], r[:, :N], ALU.mult),
                          reads=[atk[0], rtk], writes=[stk])
                    cx.dma("sp", BT[16 + h, :, tok0:tok0 + N], st[:, :N], reads=[stk])

                ctx_kl = [([kw_t[:, kt * 128:(kt + 1) * 128]], [vw_t[:, kt, :]], [kw_tk, vw_tk], None) for kt in (64, 65)]
                for (tok0, N) in qblocks:
                    sg = tok0 // NL
                    qb = (tok0 % NL) // 512
                    kl = []
                    for r_ in range(-1, 5):
                        j = qb * 4 + r_
                        if 0 <= j < 32:
                            kt, mi = sg * 32 + j, r_ + 1
                        elif j == -1:
                            kt, mi = (1 - sg) * 32 + 31, 6 + 2 * sg
                        else:
                            kt, mi = (1 - sg) * 32, 7 + 2 * sg
                        kl.append(([kw_t[:, kt * 128:(kt + 1) * 128]], [vw_t[:, kt, :]], [kw_tk, vw_tk], (mk_t[:, mi, :], mk_tk)))
                    attn(N, [q_t[:, tok0:tok0 + N]], [q_tk], kl + ctx_kl, WIN_SCALE, 1,
                         lambda accs, atk, N, tok0=tok0: finw(accs, atk, N, tok0))
                if need_ctx:
                    tok0, N = cblock
                    attn(N, [q_t[:, tok0:tok0 + N]], [q_tk], ctx_kl, WIN_SCALE, 1,
                         lambda accs, atk, N, tok0=tok0: finw(accs, atk, N, tok0))
    with cx.scope():
        DIFF_SCALE = 128.0 ** -0.5
        kr = Ring(cx, "df_k", 2, [128, TT], BF16)
        vr = Ring(cx, "df_v", 2, [128, 66, 256], BF16)
        qr = Ring(cx, "df_q", 2, [128, TT], BF16)
        dl_t, dl_tk = load_const(cx, "dlam", dlam.partition_broadcast(128), [128, 512])
        lam_t = cx.sbuf("lam", [128, 8], F32)
        lam_tk = TK()
        prod = cx.sbuf("lamprod", [128, 256], F32)
        prod_tk = TK()
        for i in range(2):
            cx.op("dve", lambda e, i=i: e.tensor_tensor(prod[:, i * 128:(i + 1) * 128], dl_t[:, 256 * i:256 * i + 128],
                                                        dl_t[:, 256 * i + 128:256 * i + 256], ALU.mult), reads=[dl_tk], writes=[prod_tk])
            cx.op("dve", lambda e, i=i: e.reduce_sum(lam_t[:, i:i + 1], prod[:, i * 128:(i + 1) * 128], axis=AX.X),
                  reads=[prod_tk], writes=[lam_tk])
        cx.op("act", lambda e: e.activation(out=lam_t[:, 2:4], in_=lam_t[:, 0:2], func=AF.Exp), reads=[lam_tk], writes=[lam_tk])
        cx.op("dve", lambda e: e.tensor_tensor(lam_t[:, 4:5], lam_t[:, 3:4], lam_t[:, 2:3], ALU.subtract), reads=[lam_tk], writes=[lam_tk])
        cx.op("dve", lambda e: e.tensor_scalar(lam_t[:, 5:6], lam_t[:, 4:5], -lam_init, None, ALU.add), reads=[lam_tk], writes=[lam_tk])
        dg_t, dg_tk = load_const(cx, "dgfm", dgfm, [128, 2])
        cx.op("dve", lambda e: e.tensor_scalar(dg_t[:], dg_t[:], 1.0 - lam_init, None, ALU.mult), reads=[dg_tk], writes=[dg_tk])
        o_sb = [[cx.sbuf("diff_o%d%d" % (m, j), [128, 512], F32) for j in range(2)] for m in range(2)]
        o_tk = [[TK(), TK()], [TK(), TK()]]
        dsq = Ring(cx, "diff_sq", 2, [128, 512], F32)
        for h in range(4):
            v_t, v_tk = vr.next()
            cx.dma("sp", v_t[:, :, :], VA[:, 1280 + h * 256:1280 + (h + 1) * 256].rearrange("(kt p) c -> p kt c", p=128), writes=[v_tk])
            kq = []
            for m in range(2):
                k_t, k_tk = load(kr, KA[11 + 2 * h + m, :, :])
                q_t, q_tk = load(qr, QT[20 + 2 * h + m, :, :])
                kq.append((k_t, k_tk, q_t, q_tk))
            for (tok0, N) in (qblocks + ([cblock] if need_ctx else [])):
                is_c = tok0 >= 2 * NL
                tiles = range(64, 66) if is_c else range(66)
                for m in range(2):
                    k_t, k_tk, q_t, q_tk = kq[m]
                    kl = [([k_t[:, kt * 128:(kt + 1) * 128]], [v_t[:, kt, 0:128], v_t[:, kt, 128:256]], [k_tk, v_tk], None) for kt in tiles]

                    def find(accs, atk, N, m=m):
                        r, rtk = rcp.next()
                        cx.op("dve", lambda e: e.reciprocal(r[:, :N], accs[2][:, :N]), reads=[atk[2]], writes=[rtk])
                        for j in range(2):
                            cx.op("dve", lambda e, j=j: e.tensor_tensor(o_sb[m][j][:, :N], accs[j][:, :N], r[:, :N], ALU.mult),
                                  reads=[atk[j], rtk], writes=[o_tk[m][j]])

                    attn(N, [q_t[:, tok0:tok0 + N]], [q_tk], kl, DIFF_SCALE, 2, find)
                pss, pstk = psS.next()
                for j in range(2):
                    cx.op("dve", lambda e, j=j: e.scalar_tensor_tensor(o_sb[0][j][:, :N], o_sb[1][j][:, :N], lam_t[:, 5:6],
                                                                       o_sb[0][j][:, :N], ALU.mult, ALU.add),
                          reads=[o_tk[1][j], o_tk[0][j], lam_tk], writes=[o_tk[0][j]])
                    sq, sqtk = dsq.next()
                    cx.op("act", lambda e, j=j, sq=sq: e.activation(out=sq[:, :N], in_=o_sb[0][j][:, :N], func=AF.Square),
                          reads=[o_tk[0][j]], writes=[sqtk])
                    cx.pe_raw(lambda pe, j=j, sq=sq: pe.matmul(pss[:, :N], ones32[:], sq[:, :N], start=(j == 0), stop=(j == 1)),
                              reads=[sqtk, ones_tk], writes=[pstk])
                r, rtk = rcp.next()
                cx.op("act", lambda e: e.activation(out=r[:, :N], in_=pss[:, :N], func=AF.Sqrt, scale=1.0 / 256, bias=EPS_T[0][:, 0:1]),
                      reads=[pstk, EPS_T[1]], writes=[rtk])
                cx.op("dve", lambda e: e.reciprocal(r[:, :N], r[:, :N]), reads=[rtk], writes=[rtk])
                for j in range(2):
                    st, stk = outst.next()
                    cx.op("dve", lambda e, j=j, st=st: e.scalar_tensor_tensor(st[:, :N], o_sb[0][j][:, :N], dg_t[:, j:j + 1],
                                                                              r[:, :N], ALU.mult, ALU.mult),
                          reads=[o_tk[0][j], rtk, dg_tk], writes=[stk])
                    cx.dma("sp", BT[24 + 2 * h + j, :, tok0:tok0 + N], st[:, :N], reads=[stk])


def emit_p3a(cx, l, G):
    need_ctx = (l == 0)
    h_in = G("h_in")
    UT = G("UT")
    BT = G("BT")
    wgate = G("wgate")
    wbr = G("wbr")
    wout = G("wout")
    wmod = G("wmod")
    brow = G("brow")
    cfm = G("cfm")
    h1 = G("h1")
    blocks = BLOCKS_ALL if need_ctx else BLOCKS_ALL[:8]
    psr = PsRing(cx, 8)
    items = []
    for v in range(2):
        for g in range(4):
            items.append((wsrc(wmod, 0, KT, g * 512, 512), KT, 512))
    for _ in blocks:
        for i in range(4):
            for fc in range(4):
                items.append((wsrc(wgate, 0, KT, i * 2048 + fc * 512, 512), KT, 512))
                items.append((wsrc(wbr, i * 1024, 8, fc * 512, 512), 8, 512))
        for fc in range(4):
            items.append((wsrc(wout, 0, KT, fc * 512, 512), KT, 512))
    ws = WStream(cx, items)
    s_t, s_tk, rep_t, rep_tk = prep_silu(cx, cfm, True)
    br_t, br_tk = load_const(cx, "brow", brow.partition_broadcast(128), [128, 2048])
    gt = [cx.sbuf("gt1_%d" % v, [128, 2048], F32) for v in range(2)]
    gt_tk = [TK(), TK()]
    for v in range(2):
        mod_rows(cx, ws, rep_t, rep_tk, br_t, br_tk, 2048, gt[v], gt_tk[v], psr, v)
    uTr = Ring(cx, "uT", 1, [128, KT, 512], BF16)
    acc = cx.sbuf("acc", [128, KT, 512], F32)
    acc_tk = [TK() for _ in range(KT)]
    mT = cx.sbuf("mT", [128, KT, 512], BF16)
    mT_tk = TK()
    btr = Ring(cx, "Bt", 2, [128, 8, 512], BF16)
    sgr = Ring(cx, "sg", 2, [128, 512], F32)
    tmr = Ring(cx, "tm", 2, [128, 512], F32)
    hpr = Ring(cx, "hp", 3, [128, 512], F32)
    yr = Ring(cx, "y1", 2, [128, 512], F32)
    outr = Ring(cx, "o1", 3, [128, 512], F32)
    for (tok0, N, var) in blocks:
        ts = slice(tok0, tok0 + N)
        uT, uT_tk = uTr.next()
        cx.dma("sp", uT[:, :, :N], UT[:, :, ts].rearrange("k p t -> p k t"), writes=[uT_tk])
        for i in range(4):
            Bt, Bt_tk = btr.next()
            cx.dma("sp", Bt[:, :, :N], BT[8 * i:8 * i + 8, :, ts].rearrange("k p t -> p k t"), writes=[Bt_tk])
            for fc in range(4):
                (wg, wgtk), (wo, wotk) = ws.get_many(2)
                for j in range(4):
                    f = fc * 4 + j
                    psg, pgtk = psr.next()
                    cx.mm(psg[:, :N], [(wg[:, k, j * 128:(j + 1) * 128], uT[:, k, :N]) for k in range(KT)],
                          reads=[wgtk, uT_tk], writes=[pgtk])
                    psy, pytk = psr.next()
                    cx.mm(psy[:, :N], [(wo[:, k, j * 128:(j + 1) * 128], Bt[:, k, :N]) for k in range(8)],
                          reads=[wotk, Bt_tk], writes=[pytk])
                    sg, sgtk = sgr.next()
                    cx.op("act", lambda e, sg=sg, psg=psg: e.activation(out=sg[:, :N], in_=psg[:, :N], func=AF.Sigmoid),
                          reads=[pgtk], writes=[sgtk])
                    if i == 0:
                        cx.op("dve", lambda e, sg=sg, psy=psy, f=f: e.tensor_tensor(acc[:, f, :N], psy[:, :N], sg[:, :N], ALU.mult),
                              reads=[pytk, sgtk], writes=[acc_tk[f]])
                    else:
                        tm, tmtk = tmr.next()
                        cx.op("dve", lambda e, sg=sg, psy=psy, tm=tm: e.tensor_tensor(tm[:, :N], psy[:, :N], sg[:, :N], ALU.mult),
                              reads=[pytk, sgtk], writes=[tmtk])
                        if i < 3:
                            cx.op("pool", lambda e, tm=tm, f=f: e.tensor_tensor(acc[:, f, :N], acc[:, f, :N], tm[:, :N], ALU.add),
                                  reads=[tmtk, acc_tk[f]], writes=[acc_tk[f]])
                        else:
                            cx.op("pool", lambda e, tm=tm, f=f: e.tensor_tensor(mT[:, f, :N], acc[:, f, :N], tm[:, :N], ALU.add),
                                  reads=[tmtk, acc_tk[f]], writes=[mT_tk])
        for fc in range(4):
            w, wtk = ws.get()
            for s in range(N // 128):
                ps, ptk = psr.next()
                cx.mm(ps[:, :], [(mT[:, k, s * 128:(s + 1) * 128], w[:, k, :]) for k in range(KT)],
                      reads=[wtk, mT_tk], writes=[ptk])
                r0 = tok0 + s * 128
                hp, hptk = hpr.next()
                cx.dma("sp", hp[:, :], h_in[r0:r0 + 128, fc * 512:(fc + 1) * 512], writes=[hptk])
                y1, y1tk = yr.next()
                cx.op("dve", lambda e, ps=ps, y1=y1, fc=fc: e.tensor_tensor(y1[:, :], ps[:, :], gt[var][:, fc * 512:(fc + 1) * 512], ALU.mult),
                      reads=[ptk, gt_tk[var]], writes=[y1tk])
                o1, o1tk = outr.next()
                cx.op("pool", lambda e, y1=y1, hp=hp, o1=o1: e.tensor_tensor(o1[:, :], y1[:, :], hp[:, :], ALU.add),
                      reads=[y1tk, hptk], writes=[o1tk])
                cx.dma("sp", h1[r0:r0 + 128, fc * 512:(fc + 1) * 512], o1[:, :], reads=[o1tk])


def emit_p3b(cx, l, G):
    need_ctx = (l == 0)
    is_moe = (l % 2 == 1)
    final = (l == 1)
    NE = N_E if is_moe else 1
    h1 = G("h1")
    wg_d = G("wg")
    wu_d = G("wu")
    wd_d = G("wd")
    wmod = G("wmod")
    bmodfm = G("bmodfm")
    brow = G("brow")
    cfm = G("cfm")
    g2fm = G("g2fm")
    ident = G("ident")
    gfin = G("gfin")
    router = G("router")
    routerb = G("routerb")
    h2 = G("h2")
    blocks = BLOCKS_ALL if need_ctx else BLOCKS_ALL[:8]
    make_eps(cx)
    ident_t, ident_tk = make_ident(cx, ident)
    psr = PsRing(cx, 8)
    items = []
    for g in range(8):
        items.append((wsrc(wmod, 0, KT, g * 512, 512), KT, 512))
    for v in range(2):
        for g in range(4):
            items.append((wsrc(wmod, 0, KT, 4096 + g * 512, 512), KT, 512))
    for _ in blocks:
        for e_ in range(NE):
            for hc in range(7):
                for half in range(2):
                    c0 = hc * 1024 + half * 512
                    items.append((wsrc(wg_d, e_ * D, KT, c0, 512), KT, 512))
                    items.append((wsrc(wu_d, e_ * D, KT, c0, 512), KT, 512))
                for ch in range(2):
                    items.append((wsrc(wd_d, e_ * DFF + hc * 1024, 8, ch * 1024, 1024), 8, 1024))
    ws = WStream(cx, items)
    modT = cx.sbuf("modT", [128, 32, 2], F32)
    modT_tk = TK()
    g2_t, g2_tk = load_const(cx, "g2fm", g2fm, [128, KT])
    gt = [cx.sbuf("gt2_%d" % v, [128, 2048], F32) for v in range(2)]
    gt_tk = [TK(), TK()]
    with cx.scope():
        s_t, s_tk, rep_t, rep_tk = prep_silu(cx, cfm, True)
        bm_t, bm_tk = load_const(cx, "bmodfm", bmodfm, [128, 32, 2])
        mod_feature_major(cx, ws, s_t, s_tk, bm_t, bm_tk, list(range(32)), modT, modT_tk, psr)
        br_t, br_tk = load_const(cx, "brow", brow.partition_broadcast(128), [128, 2048])
        for v in range(2):
            mod_rows(cx, ws, rep_t, rep_tk, br_t, br_tk, 2048, gt[v], gt_tk[v], psr, v)
    A_t, A_tk, B_t, B_tk = make_AB(cx, modT, modT_tk, 16, 0, g2_t, g2_tk, "m2")
    if final:
        gf_t, gf_tk = load_const(cx, "gfin", gfin.partition_broadcast(128), [128, 2048])
    if is_moe:
        rt_t = cx.sbuf("router", [128, KT, 8], BF16)
        rt_tk = TK()
        cx.dma("pool", rt_t[:], router.rearrange("(k p) e -> p k e", p=128), writes=[rt_tk])
        rb_t, rb_tk = load_const(cx, "routerb", routerb.partition_broadcast(128), [128, 8])
        gates = cx.sbuf("gates", [128, 4, 8], F32)
        gates_tk = TK()
        gw = cx.sbuf("gatew", [128, 4, 24], F32)
        gw_tk = TK()
    nt = NormT(cx, ident_t, ident_tk, A_t, A_tk, B_t, B_tk, psr)
    uTr = Ring(cx, "uT", 1, [128, KT, 512], BF16)
    acc2 = cx.sbuf("acc2", [128, 4, D], F32)
    acc2_tk = [TK() for _ in range(4)]
    hidr = Ring(cx, "hid", 2, [128, 8, 512], BF16)
    sgr = Ring(cx, "sg", 2, [128, 512], F32)
    st3 = Ring(cx, "fin_st", 2, [128, 4], F32)
    for (tok0, N, var) in blocks:
        nsub = N // 128
        uT, uT_tk = uTr.next()
        nt.run(h1[tok0:tok0 + N, :], N, var, uT, uT_tk)
        if is_moe:
            for s in range(nsub):
                ps, ptk = psr.next()
                cx.mm(ps[:, 0:8], [(uT[:, k, s * 128:(s + 1) * 128], rt_t[:, k, :]) for k in range(KT)],
                      reads=[uT_tk, rt_tk], writes=[ptk])
                lg = gw[:, s, 0:8]
                cx.op("dve", lambda e, ps=ps, lg=lg: e.tensor_tensor(lg, ps[:, 0:8], rb_t[:, :], ALU.add),
                      reads=[ptk, rb_tk], writes=[gw_tk])
                m1 = gw[:, s, 8:9]
                cx.op("dve", lambda e, lg=lg, m1=m1: e.reduce_max(m1, lg, axis=AX.X), reads=[gw_tk], writes=[gw_tk])
                mask1 = gw[:, s, 16:24]
                cx.op("dve", lambda e, lg=lg, m1=m1, mask1=mask1: e.tensor_scalar(mask1, lg, m1, None, ALU.is_equal),
                      reads=[gw_tk], writes=[gw_tk])
                lg2 = gates[:, s, :]
                cx.op("dve", lambda e, lg=lg, mask1=mask1, lg2=lg2: e.scalar_tensor_tensor(lg2, mask1, -1e30, lg, ALU.mult, ALU.add),
                      reads=[gw_tk], writes=[gates_tk])
                m2 = gw[:, s, 9:10]
                cx.op("dve", lambda e, lg2=lg2, m2=m2: e.reduce_max(m2, lg2, axis=AX.X), reads=[gates_tk], writes=[gw_tk])
                d_ = gw[:, s, 10:11]
                cx.op("dve", lambda e, d_=d_, m1=m1, m2=m2: e.tensor_tensor(d_, m2, m1, ALU.subtract), reads=[gw_tk], writes=[gw_tk])
                e2 = gw[:, s, 11:12]
                cx.op("act", lambda e, d_=d_, e2=e2: e.activation(out=e2, in_=d_, func=AF.Exp), reads=[gw_tk], writes=[gw_tk])
                w1 = gw[:, s, 12:13]
                cx.op("dve", lambda e, e2=e2, w1=w1: e.tensor_scalar(w1, e2, 1.0, None, ALU.add), reads=[gw_tk], writes=[gw_tk])
                cx.op("dve", lambda e, w1=w1: e.reciprocal(w1, w1), reads=[gw_tk], writes=[gw_tk])
                w2 = gw[:, s, 13:14]
                cx.op("dve", lambda e, e2=e2, w1=w1, w2=w2: e.tensor_tensor(w2, e2, w1, ALU.mult), reads=[gw_tk], writes=[gw_tk])
                cx.op("dve", lambda e, lg2=lg2, m2=m2, w2=w2: e.tensor_scalar(lg2, lg2, m2, w2, ALU.is_equal, ALU.mult),
                      reads=[gw_tk, gates_tk], writes=[gates_tk])
                cx.op("dve", lambda e, lg2=lg2, mask1=mask1, w1=w1: e.scalar_tensor_tensor(lg2, mask1, w1, lg2, ALU.mult, ALU.add),
                      reads=[gw_tk, gates_tk], writes=[gates_tk])
        for e_ in range(NE):
            for hc in range(7):
                hid, hid_tk = hidr.next()
                for half in range(2):
                    (wg, wgtk), (wu, wutk) = ws.get_many(2)
                    for j in range(4):
                        psg, pgtk = psr.next()
                        cx.mm(psg[:, :N], [(wg[:, k, j * 128:(j + 1) * 128], uT[:, k, :N]) for k in range(KT)],
                              reads=[wgtk, uT_tk], writes=[pgtk])
                        psu, putk = psr.next()
                        cx.mm(psu[:, :N], [(wu[:, k, j * 128:(j + 1) * 128], uT[:, k, :N]) for k in range(KT)],
                              reads=[wutk, uT_tk], writes=[putk])
                        sg, sgtk = sgr.next()
                        cx.op("act", lambda e, sg=sg, psg=psg: e.activation(out=sg[:, :N], in_=psg[:, :N], func=AF.Silu),
                              reads=[pgtk], writes=[sgtk])
                        cx.op("dve", lambda e, sg=sg, psu=psu, hid=hid, half=half, j=j: e.tensor_tensor(
                            hid[:, half * 4 + j, :N], psu[:, :N], sg[:, :N], ALU.mult), reads=[putk, sgtk], writes=[hid_tk])
                for ch in range(2):
                    wd, wdtk = ws.get()
                    for s in range(nsub):
                        for cc in range(2):
                            ps, ptk = psr.next()
                            cx.mm(ps[:, :], [(hid[:, k, s * 128:(s + 1) * 128], wd[:, k, cc * 512:(cc + 1) * 512]) for k in range(8)],
                                  reads=[wdtk, hid_tk], writes=[ptk])
                            dst = acc2[:, s, ch * 1024 + cc * 512:ch * 1024 + (cc + 1) * 512]
                            first = (e_ == 0 and hc == 0)
                            if not is_moe:
                                if first:
                                    cx.op("act", lambda e, ps=ps, dst=dst: e.activation(out=dst, in_=ps[:, :], func=AF.Copy),
                                          reads=[ptk], writes=[acc2_tk[s]])
                                else:
                                    cx.op("dve", lambda e, ps=ps, dst=dst: e.tensor_tensor(dst, ps[:, :], dst, ALU.add),
                                          reads=[ptk, acc2_tk[s]], writes=[acc2_tk[s]])
                            else:
                                gsc = gates[:, s, e_:e_ + 1]
                                if first:
                                    cx.op("dve", lambda e, ps=ps, dst=dst, gsc=gsc: e.tensor_scalar(dst, ps[:, :], gsc, None, ALU.mult),
                                          reads=[ptk, gates_tk], writes=[acc2_tk[s]])
                                else:
                                    cx.op("dve", lambda e, ps=ps, dst=dst, gsc=gsc: e.scalar_tensor_tensor(dst, ps[:, :], gsc, dst, ALU.mult, ALU.add),
                                          reads=[ptk, gates_tk, acc2_tk[s]], writes=[acc2_tk[s]])
        for s in range(nsub):
            r0 = tok0 + s * 128
            ht, htk = nt.hr.next()
            cx.dma("sp", ht[:], h1[r0:r0 + 128, :], writes=[htk])
            cx.op("dve", lambda e, s=s: e.tensor_tensor(acc2[:, s, :], acc2[:, s, :], gt[var][:, :], ALU.mult),
                  reads=[acc2_tk[s], gt_tk[var]], writes=[acc2_tk[s]])
            cx.op("pool", lambda e, s=s, ht=ht: e.tensor_tensor(ht[:], acc2[:, s, :], ht[:], ALU.add),
                  reads=[acc2_tk[s], htk], writes=[htk])
            if final:
                st, stk = st3.next()
                cx.op("act", lambda e, ht=ht, st=st: e.activation(out=nt.junk[:], in_=ht[:], func=AF.Square, accum_out=st[:, 0:1]),
                      reads=[htk], writes=[nt.junk_tk, stk])
                cx.op("act", lambda e, st=st: e.activation(out=st[:, 1:2], in_=st[:, 0:1], func=AF.Sqrt, scale=1.0 / D, bias=EPS_T[0][:, 0:1]),
                      reads=[stk, EPS_T[1]], writes=[stk])
                cx.op("dve", lambda e, st=st: e.reciprocal(st[:, 2:3], st[:, 1:2]), reads=[stk], writes=[stk])
                cx.op("dve", lambda e, ht=ht, st=st: e.scalar_tensor_tensor(ht[:], ht[:], st[:, 2:3], gf_t[:, :], ALU.mult, ALU.mult),
                      reads=[htk, stk, gf_tk], writes=[htk])
            cx.dma("sp", h2[r0:r0 + 128, :], ht[:], reads=[htk], final=final)


def build_fused():
    cx = Ctx()
    d = {}

    def ext(name, shape, dt=F32):
        d[name] = cx.dram(name, shape, dt, "ExternalInput")

    def scr(name, shape, dt):
        d[name] = cx.dram(name, shape, dt, "Internal")

    ext("h0", [TT, D])
    ext("cfm", [128, KT, 2])
    ext("ident", [128, 128])
    ext("rope", [4, 128, TT])
    ext("wmask", [10, 128, 512], BF16)
    ext("invcnt", [128, 4, 48])
    ext("pflag", [128, 4])
    ext("gfin", [1, 2048])
    ext("router", [D, 8])
    ext("routerb", [1, 8])
    for l in range(2):
        sfx = "_%d" % l
        ext("w1" + sfx, [D, W1_COLS])
        ext("wkvb" + sfx, [512, 2048])
        ext("wmod1" + sfx, [D, 4096])
        ext("bmodfm1" + sfx, [128, 32, 2])
        ext("g1fm" + sfx, [128, KT])
        ext("kvgfm" + sfx, [128, 4])
        ext("poolw" + sfx, [4, 256, 256])
        ext("pscfm" + sfx, [128, 8])
        ext("dlam" + sfx, [1, 512])
        ext("dgfm" + sfx, [128, 2])
        ext("sink" + sfx, [1, 8])
        ext("wgate" + sfx, [D, 8192])
        ext("wbr" + sfx, [4096, D])
        ext("wout" + sfx, [D, D])
        ext("wmodg1" + sfx, [D, 2048])
        ext("brow1" + sfx, [1, 2048])
        NE = N_E if l == 1 else 1
        ext("wg" + sfx, [NE * D, DFF])
        ext("wu" + sfx, [NE * D, DFF])
        ext("wd" + sfx, [NE * DFF, D])
        ext("wmod2" + sfx, [D, 6144])
        ext("bmodfm2" + sfx, [128, 32, 2])
        ext("brow2" + sfx, [1, 2048])
        ext("g2fm" + sfx, [128, KT])
    scr("UT", [KT, 128, TT], BF16)
    scr("QT", [NQT, 128, TT], BF16)
    scr("KT", [NKT, 128, TT], BF16)
    scr("V", [TT, VW], BF16)
    scr("AT", [8, 128, TT], BF16)
    scr("BT", [32, 128, TT], BF16)
    scr("h1", [TT, D], F32)
    scr("h2", [TT, D], F32)
    d["out"] = cx.dram("out", [NL, D], F32, "ExternalOutput")

    def mkG(l, alias):
        def G(name):
            n = alias.get(name, name)
            nl = n + "_%d" % l
            return d[nl] if nl in d else d[n]
        return G

    for l in range(2):
        hin = "h0" if l == 0 else "h2"
        with cx.scope():
            emit_p1(cx, l, mkG(l, dict(h_in=hin, wmod="wmod1", bmodfm="bmodfm1")))
        with cx.scope():
            emit_p2(cx, l, mkG(l, {}))
        with cx.scope():
            emit_p3a(cx, l, mkG(l, dict(h_in=hin, wmod="wmodg1", brow="brow1")))
        with cx.scope():
            emit_p3b(cx, l, mkG(l, dict(wmod="wmod2", bmodfm="bmodfm2", brow="brow2", h2=("h2" if l == 0 else "out"))))
    cx.finish()
    return cx.nc


def rope_tables(half):
    t = np.concatenate([np.arange(NL) + half * NL, np.arange(NL) + (1 - half) * NL])
    row = (t // 64).astype(np.float32)
    col = (t % 64).astype(np.float32)
    out = np.zeros((4, 128, TT), np.float32)
    for ti, dd in enumerate((64, 128)):
        q = dd // 4
        p = np.arange(128) % dd
        quarter = p // q
        i = (p % q).astype(np.float32)
        inv = (np.float32(10000.0) ** (-i / np.float32(q))).astype(np.float32)
        pos = np.where((quarter < 2)[:, None], row[None, :], col[None, :]).astype(np.float32)
        ang = (pos * inv[:, None]).astype(np.float32)
        sgn = np.where(quarter % 2 == 0, -1.0, 1.0).astype(np.float32)[:, None]
        out[2 * ti, :, :2 * NL] = np.cos(ang)
        out[2 * ti + 1, :, :2 * NL] = np.sin(ang) * sgn
        out[2 * ti, :, 2 * NL:] = 1.0
    return out


def _mask_tiles(half):
    k = np.arange(128)[:, None]
    q = np.arange(512)[None, :]
    m = np.zeros((10, 128, 512), np.float32)
    for r in range(-1, 5):
        m[r + 1] = (np.abs(q - k - 128 * r) <= 128)
    if half == 1:
        m[6] = m[0]
        m[9] = m[5]
    else:
        m[7] = m[5]
        m[8] = m[0]
    return m.astype(NPBF)


def _invcnt(half):
    out = np.zeros((128, 4, 48), np.float32)
    segs = ((2 * NL, half * NL, NL), (2 * NL, (1 - half) * NL, NL), (NCX, 0, NCX))
    for gi, w in enumerate((2, 4, 8, 16)):
        for seg, (n, base, ntok) in enumerate(segs):
            for ei, a0 in enumerate((0, ntok - 8)):
                t = base + a0 + np.arange(8)
                lo = np.clip(t - w // 2, 0, n)
                hi = np.clip(t - w // 2 + w, 0, n)
                out[:, gi, seg * 16 + ei * 8:seg * 16 + ei * 8 + 8] = (1.0 / (hi - lo).astype(np.float32))[None, :]
    return out


def _pflag(half):
    f = np.zeros((128, 4), np.float32)
    f[:, 0] = float(half == 1)
    f[:, 1] = float(half == 0)
    f[:, 2] = float(half == 0)
    f[:, 3] = float(half == 1)
    return f


def _cfm(inp, b):
    return np.ascontiguousarray(np.stack([fm(inp["c"][b]), fm(inp["c_ctx"])], axis=2))


def shared_inputs(inp):
    sh = dict(ident=IDENT, gfin=np.ascontiguousarray(inp["final_norm_g"].reshape(1, 2048)),
              router=np.ascontiguousarray(inp["moe_router"][0]), routerb=np.ascontiguousarray(inp["moe_router_b"][0].reshape(1, 8)))
    for l in range(2):
        s = "_%d" % l
        bm = inp["b_mod"][l]
        wm = inp["w_mod"][l]
        sh["w1" + s] = np.ascontiguousarray(inp["w_in"][l][:, _W1_IDX])
        sh["wkvb" + s] = np.ascontiguousarray(inp["mla_w_kv_b"][l][:, _KVB_IDX])
        sh["wmod1" + s] = np.ascontiguousarray(wm[:, 0:4096])
        sh["bmodfm1" + s] = np.ascontiguousarray(np.repeat(fm(bm[0:4096])[:, :, None], 2, axis=2))
        sh["g1fm" + s] = fm(inp["norm1_g"][l])
        sh["kvgfm" + s] = fm(inp["mla_kv_norm_g"][l])
        sh["poolw" + s] = np.ascontiguousarray(inp["pool_w"][l])
        sh["pscfm" + s] = fm(inp["pool_scale"][l])
        sh["dlam" + s] = np.ascontiguousarray(inp["diff_lambda"][l].reshape(1, 512))
        sh["dgfm" + s] = fm(inp["diff_subln_g"][l])
        sh["sink" + s] = np.ascontiguousarray(inp["win_sink"][l].reshape(1, 8))
        sh["wgate" + s] = np.ascontiguousarray(inp["w_in"][l][:, GATE0:])
        sh["wbr" + s] = np.concatenate([inp["pool_out"][l], inp["mla_out"][l], inp["win_out"][l], inp["diff_out"][l]], axis=0)
        sh["wout" + s] = np.ascontiguousarray(inp["w_out"][l])
        sh["wmodg1" + s] = np.ascontiguousarray(wm[:, 4096:6144])
        sh["brow1" + s] = np.ascontiguousarray(bm[4096:6144].reshape(1, 2048))
        sh["wmod2" + s] = np.ascontiguousarray(wm[:, 6144:12288])
        sh["bmodfm2" + s] = np.ascontiguousarray(np.repeat(fm(bm[6144:10240])[:, :, None], 2, axis=2))
        sh["brow2" + s] = np.ascontiguousarray(bm[10240:12288].reshape(1, 2048))
        sh["g2fm" + s] = fm(inp["norm2_g"][l])
    sh["wg_0"], sh["wu_0"], sh["wd_0"] = inp["ffn_w_gate"][0], inp["ffn_w_up"][0], inp["ffn_w_down"][0]
    sh["wg_1"] = inp["moe_w_gate"][0].reshape(N_E * D, DFF)
    sh["wu_1"] = inp["moe_w_up"][0].reshape(N_E * D, DFF)
    sh["wd_1"] = inp["moe_w_down"][0].reshape(N_E * DFF, D)
    return sh


_NC = [None]


def kernel(**inp):
    inp = {k: np.asarray(v) for k, v in inp.items()}
    sh = shared_inputs(inp)
    maps = []
    for core in range(8):
        b, half = core // 2, core % 2
        m = dict(sh)
        xb = inp["x"][b]
        m["h0"] = np.ascontiguousarray(np.concatenate(
            [xb[half * NL:(half + 1) * NL], xb[(1 - half) * NL:(2 - half) * NL], inp["ctx"][b]], axis=0))
        m["cfm"] = _cfm(inp, b)
        m["rope"] = rope_tables(half)
        m["wmask"] = _mask_tiles(half)
        m["invcnt"] = _invcnt(half)
        m["pflag"] = _pflag(half)
        maps.append(m)
    if _NC[0] is None:
        _NC[0] = build_fused()
    res = run_bass_kernel_spmd(_NC[0], maps, core_ids=list(range(8)))
    out = np.empty((4, 2 * NL, D), np.float32)
    for core in range(8):
        out[core // 2, (core % 2) * NL:(core % 2 + 1) * NL] = np.asarray(res.results[core]["out"])
    return out
```

```python
import contextlib
import math

import numpy as np
import ml_dtypes

import concourse.bass as bass
import concourse.mybir as mybir
from concourse.bass_utils import run_bass_kernel_spmd

F32 = mybir.dt.float32
BF16 = mybir.dt.bfloat16
AF = mybir.ActivationFunctionType
ALU = mybir.AluOpType
AX = mybir.AxisListType
NPBF = ml_dtypes.bfloat16

D = 2048
KT = 16
NL = 4096
NCX = 256
TT = 2 * NL + NCX
T = TT
TKEYS = TT
EPS = 1e-6
NQT = 28
NKT = 19
VW = 2304
GATE0 = 7744
N_E = 8
DFF = 7168

BLOCKS_ALL = [(i * 512, 512, 0) for i in range(16)] + [(2 * NL, 256, 1)]
BLOCKS = BLOCKS_ALL


class TK:
    __slots__ = ("w", "r", "key")

    def __init__(self):
        self.w = None
        self.r = {}
        self.key = None


class Ctx:
    def __init__(self):
        self.nc = bass.Bass("TRN2", target_bir_lowering=False)
        self.es = contextlib.ExitStack()
        nc = self.nc
        self.eng = dict(pe=nc.tensor, act=nc.scalar, dve=nc.vector, pool=nc.gpsimd, sp=nc.sync)
        self.sems = {}
        self.cnt = {}
        for e in ("pe", "act", "dve", "pool"):
            self.sems[e] = self.es.enter_context(nc.semaphore("s_" + e))
            self.cnt[e] = 0
        self.ebase = {e: 0 for e in ("pe", "act", "dve", "pool")}
        self.waited = {e: {} for e in self.eng}
        self.nid = 0
        self.finals = []
        self.nps = 0
        self.nsb = 0
        self.epoch = 0
        self.depth = 0
        self.scope_keys = []
        self.free_keys = []
        self.nsem = 0
        self.es_top = self.es
        self.bar_sem = self.es.enter_context(nc.semaphore("s_bar"))
        self.bar_cnt = 0
        self.bar_dram = nc.dram_tensor("bar_dram", [1, 16], F32, kind="Internal").ap()
        self.bar_sb = self.es.enter_context(nc.sbuf_tensor("sb_bar", [1, 16], F32))

    @contextlib.contextmanager
    def scope(self):
        old = self.es
        self.es = contextlib.ExitStack()
        self.depth += 1
        self.scope_keys.append([])
        try:
            yield
        finally:
            self.barrier()
            self.es.close()
            self.es = old
            self.depth -= 1
            keys = self.scope_keys.pop()
            if self.depth == 0:
                for k in keys:
                    if self.cnt[k] < 30000:
                        self.free_keys.append(k)
                self.epoch += 1
                for e in ("pe", "act", "dve", "pool"):
                    self.sems[e] = self.es_top.enter_context(self.nc.semaphore("s_%s_%d" % (e, self.epoch)))
                    self.cnt[e] = 0
                    for w in self.waited.values():
                        w.pop(e, None)
            else:
                self.scope_keys[-1].extend(keys)

    def barrier(self):
        for key, c in list(self.cnt.items()):
            if c > 0:
                self.wait("sp", key, c)
        self.bar_cnt += 16
        self.nc.sync.dma_start(out=self.bar_sb[:], in_=self.bar_dram).then_inc(self.bar_sem, 16)
        for e in ("pe", "act", "dve", "pool", "sp"):
            self.eng[e].wait_ge(self.bar_sem, self.bar_cnt)

    def sbuf(self, name, shape, dt):
        self.nsb += 1
        return self.es.enter_context(self.nc.sbuf_tensor("sb%d_%s" % (self.nsb, name), list(shape), dt))

    def psum(self, name=None):
        self.nps += 1
        return self.es.enter_context(self.nc.psum_tensor("ps%d" % self.nps, [128, 512], F32))

    def psum2(self):
        self.nps += 1
        return self.es.enter_context(self.nc.psum_tensor("ps%d" % self.nps, [128, 1024], F32))

    def dram(self, name, shape, dt, kind):
        return self.nc.dram_tensor(name, list(shape), dt, kind=kind).ap()

    def sem_for(self, tk):
        if tk.key is None:
            if self.free_keys:
                tk.key = self.free_keys.pop()
            else:
                tk.key = ("d", self.nid)
                self.nid += 1
                self.sems[tk.key] = self.es_top.enter_context(self.nc.semaphore("d%d" % self.nid))
                self.cnt[tk.key] = 0
            if self.scope_keys:
                self.scope_keys[-1].append(tk.key)
        return tk.key

    def wait(self, e, key, val):
        if isinstance(key, tuple) and key[0] == "e":
            if key[2] != self.epoch:
                return
            key = key[1]
        if self.waited[e].get(key, 0) >= val:
            return
        self.waited[e][key] = val
        self.eng[e].wait_ge(self.sems[key], val)

    def deps(self, e, reads, writes):
        d = {}

        def add(ev):
            if ev is None:
                return
            k, v = ev
            if d.get(k, 0) < v:
                d[k] = v

        for t in reads:
            add(t.w)
        for t in writes:
            add(t.w)
            for k, v in t.r.items():
                if not (isinstance(k, tuple) and k[0] == "e" and k[1] == e):
                    add((k, v))
        for k, v in d.items():
            if isinstance(k, tuple) and k[0] == "e" and k[1] == e and e == "pe":
                continue
            self.wait(e, k, v)

    def done(self, ev, reads, writes):
        k, v = ev
        for t in reads:
            if t.r.get(k, 0) < v:
                t.r[k] = v
        for t in writes:
            t.w = ev
            t.r = {}

    def op(self, e, fn, reads=(), writes=()):
        self.deps(e, reads, writes)
        ins = fn(self.eng[e])
        self.cnt[e] += 1
        ins.then_inc(self.sems[e], 1)
        self.done((("e", e, self.epoch), self.cnt[e]), reads, writes)
        return ins

    def mm(self, out, pairs, reads=(), writes=()):
        self.deps("pe", reads, writes)
        n = len(pairs)
        ins = None
        for i, (l, r) in enumerate(pairs):
            ins = self.nc.tensor.matmul(out, l, r, start=(i == 0), stop=(i == n - 1))
        self.cnt["pe"] += 1
        ins.then_inc(self.sems["pe"], 1)
        self.done((("e", "pe", self.epoch), self.cnt["pe"]), reads, writes)

    def pe_raw(self, emit, reads=(), writes=()):
        self.deps("pe", reads, writes)
        ins = emit(self.nc.tensor)
        self.cnt["pe"] += 1
        ins.then_inc(self.sems["pe"], 1)
        self.done((("e", "pe", self.epoch), self.cnt["pe"]), reads, writes)

    def dma(self, e, out, in_, reads=(), writes=(), final=False):
        self.deps(e, reads, writes)
        tk = writes[0] if writes else reads[0]
        key = self.sem_for(tk)
        ins = self.eng[e].dma_start(out=out, in_=in_)
        self.cnt[key] += 16
        ins.then_inc(self.sems[key], 16)
        self.done((key, self.cnt[key]), reads, writes)
        if final:
            self.finals.append((key, self.cnt[key]))

    def finish(self):
        for key, v in self.finals:
            self.wait("sp", key, v)
        for e in ("pe", "act", "dve", "pool"):
            if self.cnt[e] > 0:
                self.wait("sp", e, self.cnt[e])


class Ring:
    def __init__(self, cx, name, n, shape, dt):
        self.t = [cx.sbuf("%s%d" % (name, i), shape, dt) for i in range(n)]
        self.tk = [TK() for _ in range(n)]
        self.i = 0
        self.n = n

    def next(self):
        j = self.i % self.n
        self.i += 1
        return self.t[j], self.tk[j]


class PsRing:
    def __init__(self, cx, n):
        self.t = [cx.psum() for _ in range(n)]
        self.tk = [TK() for _ in range(n)]
        self.i = 0
        self.n = n

    def next(self):
        j = self.i % self.n
        self.i += 1
        return self.t[j], self.tk[j]


class WStream:
    def __init__(self, cx, items, nslots=4, slot=8192, eng="pool"):
        self.cx = cx
        self.items = items
        self.ring = Ring(cx, "wring", nslots, [128, slot], BF16)
        self.issued = []
        self.u = 0
        self.eng = eng

    def _issue(self):
        j = len(self.issued)
        src, a, b = self.items[j]
        t, tk = self.ring.next()
        view = t[:, 0:a * b].rearrange("p (a b) -> p a b", a=a)
        self.cx.dma(self.eng, view, src, writes=[tk])
        self.issued.append((view, tk))

    def get_many(self, m):
        assert m <= self.ring.n
        while len(self.issued) < min(len(self.items), self.u + self.ring.n):
            self._issue()
        r = self.issued[self.u:self.u + m]
        self.u += m
        return r

    def get(self):
        return self.get_many(1)[0]


def wsrc(w2d, r0, nk, c0, nc_):
    return w2d[r0:r0 + nk * 128, c0:c0 + nc_].rearrange("(k p) c -> p k c", p=128)


def load_const(cx, name, src, shape, dt=F32, eng="sp"):
    t = cx.sbuf(name, shape, dt)
    tk = TK()
    cx.dma(eng, t[:], src, writes=[tk])
    return t, tk


def mod_feature_major(cx, ws, silu_t, silu_tk, bmod_t, bmod_tk, col_tiles, out_t, out_tk, psr):
    for g0 in range(0, len(col_tiles), 4):
        w, wtk = ws.get()
        for jj in range(4):
            j = g0 + jj
            ct = col_tiles[j]
            ps, ptk = psr.next()
            cx.mm(ps[:, 0:2], [(w[:, k, jj * 128:(jj + 1) * 128], silu_t[:, k, :]) for k in range(KT)],
                  reads=[wtk, silu_tk], writes=[ptk])
            cx.op("dve", lambda e, ps=ps, j=j, ct=ct: e.tensor_tensor(
                out_t[:, j, :], ps[:, 0:2], bmod_t[:, ct, :], ALU.add),
                reads=[ptk, bmod_tk], writes=[out_tk])


def bmod2(bmod_t, ct):
    return bmod_t[:, ct, :]


def mod_rows(cx, ws, silurep_t, silurep_tk, brow_t, brow_tk, ncol, out_t, out_tk, psr, variant):
    for g in range(ncol // 512):
        w, wtk = ws.get()
        ps, ptk = psr.next()
        cx.mm(ps[:, :], [(silurep_t[:, variant, k, :], w[:, k, :]) for k in range(KT)],
              reads=[wtk, silurep_tk], writes=[ptk])
        cx.op("dve", lambda e, ps=ps, g=g: e.tensor_tensor(out_t[:, g * 512:(g + 1) * 512], ps[:, :],
                                                           brow_t[:, g * 512:(g + 1) * 512], ALU.add),
              reads=[ptk, brow_tk], writes=[out_tk])


def prep_silu(cx, cfm_ap, need_rep):
    c_t, c_tk = load_const(cx, "cfm", cfm_ap, [128, KT, 2])
    s_t = cx.sbuf("silu_c", [128, KT, 2], BF16)
    s_tk = TK()
    cx.op("act", lambda e: e.activation(out=s_t[:], in_=c_t[:], func=AF.Silu), reads=[c_tk], writes=[s_tk])
    rep_t = rep_tk = None
    if need_rep:
        s32 = cx.sbuf("silu_c32", [128, KT, 2], F32)
        s32_tk = TK()
        cx.op("act", lambda e: e.activation(out=s32[:], in_=c_t[:], func=AF.Silu), reads=[c_tk], writes=[s32_tk])
        ones = cx.sbuf("ones_rep", [128, 128], F32)
        o_tk = TK()
        cx.op("dve", lambda e: e.memset(ones[:], 1.0), writes=[o_tk])
        rep_t = cx.sbuf("silu_rep", [128, 2, KT, 128], BF16)
        rep_tk = TK()
        for v in range(2):
            for k in range(KT):
                cx.op("act", lambda e, v=v, k=k: e.activation(out=rep_t[:, v, k, :], in_=ones[:], func=AF.Copy,
                                                              scale=s32[:, k, v:v + 1]),
                      reads=[s32_tk, o_tk], writes=[rep_tk])
    return s_t, s_tk, rep_t, rep_tk


class NormT:
    def __init__(self, cx, ident_t, ident_tk, A_t, A_tk, B_t, B_tk, psr):
        self.cx = cx
        self.hr = Ring(cx, "nt_h", 2, [128, D], F32)
        self.un = Ring(cx, "nt_un", 2, [128, D], F32)
        self.junk = cx.sbuf("nt_junk", [128, D], BF16)
        self.junk_tk = TK()
        self.st = Ring(cx, "nt_st", 4, [128, 4], F32)
        self.ident_t, self.ident_tk = ident_t, ident_tk
        self.A_t, self.A_tk, self.B_t, self.B_tk = A_t, A_tk, B_t, B_tk
        self.psr = psr
        self.flip = 0

    def run(self, h_ap, N, variant, uT, uT_tk, post=None):
        cx = self.cx
        for s in range(N // 128):
            ht, htk = self.hr.next()
            cx.dma("sp", ht[:], h_ap[s * 128:(s + 1) * 128, :], writes=[htk])
            st, stk = self.st.next()
            cx.op("act", lambda e: e.activation(out=self.junk[:], in_=ht[:], func=AF.Square, accum_out=st[:, 0:1]),
                  reads=[htk], writes=[self.junk_tk, stk])
            cx.op("act", lambda e: e.activation(out=st[:, 1:2], in_=st[:, 0:1], func=AF.Sqrt, scale=1.0 / D, bias=EPS_T[0][:, 0:1]),
                  reads=[stk, EPS_T[1]], writes=[stk])
            cx.op("dve", lambda e: e.reciprocal(st[:, 2:3], st[:, 1:2]), reads=[stk], writes=[stk])
            un, untk = self.un.next()
            cx.op("act", lambda e: e.activation(out=un[:], in_=ht[:], func=AF.Copy, scale=st[:, 2:3]),
                  reads=[htk, stk], writes=[untk])
            for kg in range(4):
                ps, ptk = self.psr.next()

                def emit(pe, ps=ps, kg=kg, un=un):
                    ins = None
                    for kk in range(4):
                        k = kg * 4 + kk
                        ins = pe.transpose(ps[:, kk * 128:(kk + 1) * 128], un[:, k * 128:(k + 1) * 128], self.ident_t[:])
                    return ins

                cx.pe_raw(emit, reads=[untk, self.ident_tk], writes=[ptk])
                for kk in range(4):
                    k = kg * 4 + kk
                    self.flip ^= 1
                    if self.flip:
                        cx.op("act", lambda e, k=k, kk=kk, ps=ps: e.activation(
                            out=uT[:, k, s * 128:(s + 1) * 128], in_=ps[:, kk * 128:(kk + 1) * 128], func=AF.Identity,
                            scale=self.A_t[:, k, variant:variant + 1], bias=self.B_t[:, k, variant:variant + 1]),
                            reads=[ptk, self.A_tk, self.B_tk], writes=[uT_tk])
                    else:
                        cx.op("dve", lambda e, k=k, kk=kk, ps=ps: e.tensor_scalar(
                            uT[:, k, s * 128:(s + 1) * 128], ps[:, kk * 128:(kk + 1) * 128],
                            self.A_t[:, k, variant:variant + 1], self.B_t[:, k, variant:variant + 1], ALU.mult, ALU.add),
                            reads=[ptk, self.A_tk, self.B_tk], writes=[uT_tk])


EPS_T = [None, None]


def make_eps(cx, val=EPS):
    t = cx.sbuf("eps_c", [128, 1], F32)
    tk = TK()
    cx.op("dve", lambda e: e.memset(t[:], val), writes=[tk])
    EPS_T[0], EPS_T[1] = t, tk


def make_ident(cx, ident_ap):
    return load_const(cx, "ident", ident_ap, [128, 128])


def make_AB(cx, modT, modT_tk, j_sc, j_sh, gfm_t, gfm_tk, name):
    A = cx.sbuf(name + "_A", [128, KT, 2], F32)
    B = cx.sbuf(name + "_B", [128, KT, 2], F32)
    atk, btk = TK(), TK()
    cx.op("dve", lambda e: e.tensor_scalar(A[:], modT[:, j_sc:j_sc + KT, :], 1.0, None, ALU.add),
          reads=[modT_tk], writes=[atk])
    for v in range(2):
        cx.op("dve", lambda e, v=v: e.tensor_tensor(A[:, :, v], A[:, :, v], gfm_t[:, :], ALU.mult),
              reads=[gfm_tk, atk], writes=[atk])
    cx.op("dve", lambda e: e.tensor_copy(B[:], modT[:, j_sh:j_sh + KT, :]), reads=[modT_tk], writes=[btk])
    return A, atk, B, btk


ROPE_TILES = (
    [("q", 8 + j, 0) for j in range(4)] + [("k", 8, 0)]
    + [("q", 12 + h, 1) for h in range(8)] + [("k", 9 + h, 1) for h in range(2)]
    + [("q", 20 + j, 1) for j in range(8)] + [("k", 11 + j, 1) for j in range(8)]
)
W1_COLS = 5 * 512 + len(ROPE_TILES) * 256 + 1280


def w1_col_index():
    idx = []
    idx += list(range(0, 1024))
    for h in range(8):
        idx += list(range(1024 + 192 * h, 1024 + 192 * h + 128))
    idx += list(range(2560, 3072))

    def perm(cols, d):
        q = d // 4
        out = []
        for b0 in range(0, len(cols), d):
            blk = cols[b0:b0 + d]
            out += blk[q:2 * q] + blk[0:q] + blk[3 * q:4 * q] + blk[2 * q:3 * q]
        return out

    ropes = []
    for j in range(4):
        c = []
        for h in (2 * j, 2 * j + 1):
            c += list(range(1024 + 192 * h + 128, 1024 + 192 * h + 192))
        ropes.append((c, 64))
    ropes.append((list(range(3072, 3136)) * 2, 64))
    for h in range(8):
        ropes.append((list(range(3136 + 128 * h, 3136 + 128 * h + 128)), 128))
    for h in range(2):
        ropes.append((list(range(4160 + 128 * h, 4160 + 128 * h + 128)), 128))
    for j in range(8):
        ropes.append((list(range(4672 + 128 * j, 4672 + 128 * j + 128)), 128))
    for j in range(8):
        ropes.append((list(range(5696 + 128 * j, 5696 + 128 * j + 128)), 128))
    assert len(ropes) == len(ROPE_TILES)
    for c, d in ropes:
        idx += c + perm(c, d)
    idx += list(range(4416, 4672))
    idx += list(range(6720, 7744))
    assert len(idx) == W1_COLS
    return np.asarray(idx, dtype=np.int64)


def kvb_col_index():
    idx = []
    for h in range(8):
        idx += list(range(256 * h, 256 * h + 128))
    for h in range(8):
        idx += list(range(256 * h + 128, 256 * h + 256))
    return np.asarray(idx, dtype=np.int64)


def emit_p1(cx, l, G):
    h_in = G("h_in")
    w1 = G("w1")
    wkvb = G("wkvb")
    wmod = G("wmod")
    bmodfm = G("bmodfm")
    cfm = G("cfm")
    g1fm = G("g1fm")
    kvgfm = G("kvgfm")
    ident = G("ident")
    rope = G("rope")
    UT = G("UT")
    QT = G("QT")
    KTo = G("KT")
    Vo = G("V")
    AT = G("AT")

    make_eps(cx)
    ident_t, ident_tk = make_ident(cx, ident)
    psr = PsRing(cx, 8)

    items = []
    for g in range(8):
        items.append((wsrc(wmod, 0, KT, g * 512, 512), KT, 512))
    for (tok0, N, var) in BLOCKS:
        c0 = 0
        for g in range(5):
            items.append((wsrc(w1, 0, KT, c0, 512), KT, 512))
            c0 += 512
        items.append((wsrc(wkvb, 0, 4, 0, 2048), 4, 2048))
        nr = len(ROPE_TILES)
        for g in range((nr + 1) // 2):
            ncol = 512 if 2 * g + 1 < nr else 256
            items.append((wsrc(w1, 0, KT, c0, ncol), KT, ncol))
            c0 += ncol
        for ncol in (512, 512, 256):
            items.append((wsrc(w1, 0, KT, c0, ncol), KT, ncol))
            c0 += ncol
        assert c0 == W1_COLS
    ws = WStream(cx, items)

    s_t, s_tk, _, _ = prep_silu(cx, cfm, False)
    bm_t, bm_tk = load_const(cx, "bmodfm", bmodfm, [128, 32, 2])
    g1_t, g1_tk = load_const(cx, "g1fm", g1fm, [128, KT])
    kvg_t, kvg_tk = load_const(cx, "kvgfm", kvgfm, [128, 4])
    modT = cx.sbuf("modT", [128, 32, 2], F32)
    modT_tk = TK()
    mod_feature_major(cx, ws, s_t, s_tk, bm_t, bm_tk, list(range(32)), modT, modT_tk, psr)
    A_t, A_tk, B_t, B_tk = make_AB(cx, modT, modT_tk, 16, 0, g1_t, g1_tk, "m1")
    nt = NormT(cx, ident_t, ident_tk, A_t, A_tk, B_t, B_tk, psr)

    ones32 = cx.sbuf("ones32", [128, 128], F32)
    ones_tk = TK()
    cx.op("dve", lambda e: e.memset(ones32[:], 1.0), writes=[ones_tk])

    uTr = Ring(cx, "uT", 2, [128, KT, 512], BF16)
    stg = Ring(cx, "stg", 6, [128, 512], BF16)
    vstg = Ring(cx, "vstg", 3, [128, 1024], BF16)
    ropeR = Ring(cx, "ropeR", 2, [128, 4, 512], F32)
    t1r = Ring(cx, "t1r", 2, [128, 512], F32)
    t2r = Ring(cx, "t2r", 2, [128, 512], F32)
    ckv_t = cx.sbuf("ckv", [128, 4, 512], F32)
    ckv_tk = TK()
    ckvsq = Ring(cx, "ckvsq", 2, [128, 512], F32)
    rs_t = cx.sbuf("ckv_rs", [128, 512], F32)
    rs_tk = TK()
    ckvn_t = cx.sbuf("ckvn", [128, 4, 512], BF16)
    ckvn_tk = TK()
    flip = [0]

    def evac_copy(ps, ptk, N, dst_ap):
        st, stk = stg.next()
        flip[0] ^= 1
        if flip[0]:
            cx.op("act", lambda e: e.activation(out=st[:, :N], in_=ps[:, :N], func=AF.Copy), reads=[ptk], writes=[stk])
        else:
            cx.op("dve", lambda e: e.tensor_copy(st[:, :N], ps[:, :N]), reads=[ptk], writes=[stk])
        cx.dma("sp", dst_ap, st[:, :N], reads=[stk])

    def blk_pro(tok0, N, var):
        ts = slice(tok0, tok0 + N)
        uT, uT_tk = uTr.next()
        nt.run(h_in[tok0:tok0 + N, :], N, var, uT, uT_tk)
        cx.dma("sp", UT[:, :, ts].rearrange("k p t -> p k t"), uT[:, :, :N], reads=[uT_tk])
        rp, rptk = ropeR.next()
        cx.dma("sp", rp[:, :, :N], rope[:, :, ts].rearrange("c p t -> p c t"), writes=[rptk])

        return uT, uT_tk, rp, rptk

    def blk_body(tok0, N, var, uT, uT_tk, rp, rptk, next_pro):
        ts = slice(tok0, tok0 + N)
        def proj_tile(w, wtk, j):
            ps, ptk = psr.next()
            cx.mm(ps[:, :N], [(w[:, k, j * 128:(j + 1) * 128], uT[:, k, :N]) for k in range(KT)],
                  reads=[wtk, uT_tk], writes=[ptk])
            return ps, ptk

        for g in range(4):
            w, wtk = ws.get()
            for j in range(4):
                ps, ptk = proj_tile(w, wtk, j)
                tile_i = (g % 2) * 4 + j
                dst = AT[tile_i, :, ts] if g < 2 else QT[tile_i, :, ts]
                evac_copy(ps, ptk, N, dst)
        w, wtk = ws.get()
        for j in range(4):
            ps, ptk = proj_tile(w, wtk, j)
            cx.op("act", lambda e, ps=ps, j=j: e.activation(out=ckv_t[:, j, :N], in_=ps[:, :N], func=AF.Copy),
                  reads=[ptk], writes=[ckv_tk])
        pss, pstk = psr.next()
        sqs = []
        for j in range(4):
            sq, sqtk = ckvsq.next()
            cx.op("act", lambda e, sq=sq, j=j: e.activation(out=sq[:, :N], in_=ckv_t[:, j, :N], func=AF.Square),
                  reads=[ckv_tk], writes=[sqtk])
            cx.pe_raw(lambda pe, sq=sq, j=j: pe.matmul(pss[:, :N], ones32[:], sq[:, :N], start=(j == 0), stop=(j == 3)),
                      reads=[sqtk, ones_tk], writes=[pstk])
        cx.op("act", lambda e: e.activation(out=rs_t[:, :N], in_=pss[:, :N], func=AF.Sqrt, scale=1.0 / 512, bias=EPS_T[0][:, 0:1]),
              reads=[pstk, EPS_T[1]], writes=[rs_tk])
        cx.op("dve", lambda e: e.reciprocal(rs_t[:, :N], rs_t[:, :N]), reads=[rs_tk], writes=[rs_tk])
        for j in range(4):
            cx.op("dve", lambda e, j=j: e.scalar_tensor_tensor(ckvn_t[:, j, :N], ckv_t[:, j, :N], kvg_t[:, j:j + 1],
                                                               rs_t[:, :N], ALU.mult, ALU.mult),
                  reads=[ckv_tk, rs_tk, kvg_tk], writes=[ckvn_tk])
        w, wtk = ws.get()
        for h in range(8):
            ps, ptk = psr.next()
            cx.mm(ps[:, :N], [(w[:, k, h * 128:(h + 1) * 128], ckvn_t[:, k, :N]) for k in range(4)],
                  reads=[wtk, ckvn_tk], writes=[ptk])
            evac_copy(ps, ptk, N, KTo[h, :, ts])
        for s in range(N // 128):
            vs, vstk = vstg.next()
            for c in range(2):
                ps, ptk = psr.next()
                cx.mm(ps[:, :], [(ckvn_t[:, k, s * 128:(s + 1) * 128], w[:, k, 1024 + c * 512:1024 + (c + 1) * 512]) for k in range(4)],
                      reads=[wtk, ckvn_tk], writes=[ptk])
                cx.op("act", lambda e, ps=ps, c=c, vs=vs: e.activation(out=vs[:, c * 512:(c + 1) * 512], in_=ps[:, :], func=AF.Copy),
                      reads=[ptk], writes=[vstk])
            cx.dma("sp", Vo[tok0 + s * 128:tok0 + (s + 1) * 128, 0:1024], vs[:, :], reads=[vstk])
        nr = len(ROPE_TILES)
        for g in range((nr + 1) // 2):
            w, wtk = ws.get()
            for jj in range(2):
                ri = 2 * g + jj
                if ri >= nr:
                    break
                dest, dtile, tabt = ROPE_TILES[ri]
                psx, ptkx = proj_tile(w, wtk, 2 * jj)
                psp, ptkp = proj_tile(w, wtk, 2 * jj + 1)
                t1, t1k = t1r.next()
                t2, t2k = t2r.next()
                cx.op("dve", lambda e, psx=psx, t1=t1, tabt=tabt: e.tensor_tensor(t1[:, :N], psx[:, :N], rp[:, 2 * tabt, :N], ALU.mult),
                      reads=[ptkx, rptk], writes=[t1k])
                cx.op("dve", lambda e, psp=psp, t2=t2, tabt=tabt: e.tensor_tensor(t2[:, :N], psp[:, :N], rp[:, 2 * tabt + 1, :N], ALU.mult),
                      reads=[ptkp, rptk], writes=[t2k])
                st, stk = stg.next()
                cx.op("pool", lambda e, st=st, t1=t1, t2=t2: e.tensor_tensor(st[:, :N], t1[:, :N], t2[:, :N], ALU.add),
                      reads=[t1k, t2k], writes=[stk])
                dst = QT[dtile, :, ts] if dest == "q" else KTo[dtile, :, ts]
                cx.dma("sp", dst, st[:, :N], reads=[stk])
        next_pro()
        wl = ws.get_many(3)
        for s in range(N // 128):
            vs, vstk = vstg.next()
            vs2, vstk2 = vstg.next()
            for gi, (w, wtk) in enumerate(wl):
                ncol = 512 if gi < 2 else 256
                ps, ptk = psr.next()
                cx.mm(ps[:, :ncol], [(uT[:, k, s * 128:(s + 1) * 128], w[:, k, :ncol]) for k in range(KT)],
                      reads=[wtk, uT_tk], writes=[ptk])
                if gi == 0:
                    cx.op("act", lambda e, ps=ps, vs=vs: e.activation(out=vs[:, 0:512], in_=ps[:, :], func=AF.Copy),
                          reads=[ptk], writes=[vstk])
                elif gi == 1:
                    cx.op("dve", lambda e, ps=ps, vs=vs: e.tensor_copy(vs[:, 512:1024], ps[:, :]), reads=[ptk], writes=[vstk])
                else:
                    cx.op("act", lambda e, ps=ps, vs2=vs2: e.activation(out=vs2[:, 0:256], in_=ps[:, :256], func=AF.Copy),
                          reads=[ptk], writes=[vstk2])
            r0 = tok0 + s * 128
            cx.dma("sp", Vo[r0:r0 + 128, 1024:2048], vs[:, :], reads=[vstk])
            cx.dma("sp", Vo[r0:r0 + 128, 2048:2304], vs2[:, 0:256], reads=[vstk2])

    st_ = [blk_pro(*BLOCKS[0])]
    for bi, (tok0, N, var) in enumerate(BLOCKS):
        cur = st_[0]

        def next_pro(bi=bi):
            if bi + 1 < len(BLOCKS):
                st_[0] = blk_pro(*BLOCKS[bi + 1])

        blk_body(tok0, N, var, cur[0], cur[1], cur[2], cur[3], next_pro)


def fm(v):
    return np.ascontiguousarray(v.reshape(-1, 128).T)


_W1_IDX = w1_col_index()
_KVB_IDX = kvb_col_index()
IDENT = np.eye(128, dtype=np.float32)


HL = 128 + NL + 128
PL = 8 + NL + 8
PLC = 8 + NCX + 8


def emit_p2(cx, l, G):
    need_ctx = (l == 0)
    lam_init = 0.8 - 0.6 * math.exp(-0.3 * l)
    QT = G("QT")
    KA = G("KT")
    VA = G("V")
    wmask = G("wmask")
    AT = G("AT")
    pflag = G("pflag")
    invcnt = G("invcnt")
    poolw = G("poolw")
    pscfm = G("pscfm")
    dlam = G("dlam")
    dgfm = G("dgfm")
    sink = G("sink")
    BT = G("BT")

    make_eps(cx)
    psS = PsRing(cx, 1)
    psA = [PsRing(cx, 3)]
    psS2_t = [cx.psum2() for _ in range(2)]
    psS2_tk = [TK(), TK()]
    psS2_i = [0]
    ones_bf = cx.sbuf("ones_bf", [128, 128], BF16)
    ones_bf_tk = TK()
    cx.op("dve", lambda e: e.memset(ones_bf[:], 1.0), writes=[ones_bf_tk])
    ones32 = cx.sbuf("ones32", [128, 128], F32)
    ones_tk = TK()
    cx.op("dve", lambda e: e.memset(ones32[:], 1.0), writes=[ones_tk])
    pTr = Ring(cx, "pT", 3, [128, 1024], BF16)
    outst = Ring(cx, "outst", 4, [128, 512], BF16)
    rcp = Ring(cx, "rcp", 2, [128, 512], F32)
    setflip = [0]

    qblocks = [(i * 512, 512) for i in range(16 if need_ctx else 8)]
    cblock = (2 * NL, 256)

    sacc_r = Ring(cx, "sacc", 2, [128, 1024], F32)
    LOOK = 2

    def attn(N, q_parts, q_tks, kt_list, scale, nO, finish, mask_of=None):
        aset = psA[0]
        accs = [aset.t[j] for j in range(3)]
        atk = aset.tk
        n = len(kt_list)
        assert n % 2 == 0
        npair = n // 2
        sacc, sacc_tk = sacc_r.next()
        stages = []

        def v3(t):
            return t[:, :].rearrange("p (t n) -> p t n", t=2)[:, :, :N]

        def issue_S(p):
            pair = kt_list[2 * p:2 * p + 2]
            j = psS2_i[0] % 2
            psS2_i[0] += 1
            ps, ptk = psS2_t[j], psS2_tk[j]
            alltk = list(q_tks)
            for (_, _, tks, _) in pair:
                alltk += list(tks)

            def emit(pe, ps=ps, pair=pair):
                ins = None
                for t, (kparts, vparts, tks, mask) in enumerate(pair):
                    nk = len(kparts)
                    for ci in range(nk):
                        ins = pe.matmul(ps[:, t * 512:t * 512 + N], kparts[ci], q_parts[ci], start=(ci == 0), stop=(ci == nk - 1))
                return ins

            cx.pe_raw(emit, reads=alltk, writes=[ptk])
            pt, pttk = pTr.next()
            cx.op("act", lambda e, ps=ps, pt=pt: e.activation(out=v3(pt), in_=v3(ps), func=AF.Exp, scale=scale),
                  reads=[ptk], writes=[pttk])
            for t, (_, _, _, mask) in enumerate(pair):
                if mask is not None:
                    mk_ap, mk_tk = mask
                    cx.op("pool", lambda e, pt=pt, mk_ap=mk_ap, t=t: e.tensor_tensor(
                        pt[:, t * 512:t * 512 + N], pt[:, t * 512:t * 512 + N], mk_ap[:, :N], ALU.mult),
                        reads=[mk_tk, pttk], writes=[pttk])
            if p == 0:
                cx.op("dve", lambda e, pt=pt: e.tensor_copy(v3(sacc), v3(pt)), reads=[pttk], writes=[sacc_tk])
            else:
                cx.op("dve", lambda e, pt=pt: e.tensor_tensor(v3(sacc), v3(sacc), v3(pt), ALU.add),
                      reads=[pttk, sacc_tk], writes=[sacc_tk])
            stages.append((pt, pttk, pair, alltk))

        def issue_PV(p):
            pt, pttk, pair, alltk = stages[p]

            def emit(pe, pt=pt, pair=pair, p=p):
                ins = None
                for t, (kparts, vparts, tks, mask) in enumerate(pair):
                    for j in range(nO):
                        ins = pe.matmul(accs[j][:, :N], vparts[j], pt[:, t * 512:t * 512 + N],
                                        start=(p == 0 and t == 0), stop=(p == npair - 1 and t == 1))
                return ins

            cx.pe_raw(emit, reads=[pttk] + alltk, writes=[atk[j] for j in range(nO)])

        for i in range(npair + LOOK):
            if i < npair:
                issue_S(i)
            if i - LOOK >= 0:
                issue_PV(i - LOOK)

        def emit_sum(pe):
            pe.matmul(accs[2][:, :N], ones32[:], sacc[:, 0:N], start=True, stop=False)
            return pe.matmul(accs[2][:, :N], ones32[:], sacc[:, 512:512 + N], start=False, stop=True)

        cx.pe_raw(emit_sum, reads=[sacc_tk, ones_tk], writes=[atk[2]])
        finish(accs, atk, N)

    def load(ring, src, shape_view=None):
        t, tk = ring.next()
        dst = t[:] if shape_view is None else shape_view(t)
        cx.dma("sp", dst, src, writes=[tk])
        return t, tk

    with cx.scope():
        pw_t = cx.sbuf("poolw", [128, 8, 256], BF16)
        pw_tk = TK()
        cx.dma("pool", pw_t[:], poolw.rearrange("g (k p) d -> p (g k) d", p=128), writes=[pw_tk])
        psc_t, psc_tk = load_const(cx, "pscfm", pscfm, [128, 8])
        inv_t, inv_tk = load_const(cx, "invcnt", invcnt, [128, 4, 48])
        pf_t, pf_tk = load_const(cx, "pflag", pflag, [128, 4])
        xr = Ring(cx, "pool_x", 2, [128, PL], BF16)
        sA = cx.sbuf("pool_sA", [128, PL], F32)
        sB = cx.sbuf("pool_sB", [128, PL], F32)
        sA_tk, sB_tk = TK(), TK()
        pooled = [cx.sbuf("pooled%d" % i, [128, TT], BF16) for i in range(2)]
        pooled_tk = [TK(), TK()]
        for gi, w in enumerate((2, 4, 8, 16)):
            for seg in ((0, 1, 2) if need_ctx else (0,)):
                n_tok = NCX if seg == 2 else NL
                L = n_tok + 16
                o0 = seg * NL
                oth = (1 - seg) * NL
                for k in range(2):
                    x, xtk = xr.next()
                    src = AT[2 * gi + k, :, :]
                    cx.dma("sp", x[:, 8:8 + n_tok], src[:, o0:o0 + n_tok], writes=[xtk])
                    if seg == 2:
                        cx.op("dve", lambda e, x=x: e.memset(x[:, 0:8], 0.0), writes=[xtk])
                        cx.op("dve", lambda e, x=x, n_tok=n_tok: e.memset(x[:, 8 + n_tok:16 + n_tok], 0.0), writes=[xtk])
                    else:
                        cx.dma("sp", x[:, 0:8], src[:, oth + NL - 8:oth + NL], writes=[xtk])
                        cx.dma("sp", x[:, 8 + n_tok:16 + n_tok], src[:, oth:oth + 8], writes=[xtk])
                        cx.op("dve", lambda e, x=x, seg=seg: e.tensor_scalar(x[:, 0:8], x[:, 0:8], pf_t[:, 2 * seg:2 * seg + 1], None, ALU.mult),
                              reads=[pf_tk], writes=[xtk])
                        cx.op("dve", lambda e, x=x, seg=seg, n_tok=n_tok: e.tensor_scalar(
                            x[:, 8 + n_tok:16 + n_tok], x[:, 8 + n_tok:16 + n_tok], pf_t[:, 2 * seg + 1:2 * seg + 2], None, ALU.mult),
                              reads=[pf_tk], writes=[xtk])
                    cur, cur_tk, ln = x, xtk, L
                    step = 1
                    bufs = [(sA, sA_tk), (sB, sB_tk)]
                    bi = 0
                    while step < w:
                        dst, dst_tk = bufs[bi]
                        bi ^= 1
                        ln2 = ln - step
                        cx.op("dve", lambda e, dst=dst, cur=cur, ln2=ln2, step=step: e.tensor_tensor(
                            dst[:, :ln2], cur[:, :ln2], cur[:, step:step + ln2], ALU.add), reads=[cur_tk], writes=[dst_tk])
                        cur, cur_tk, ln = dst, dst_tk, ln2
                        step *= 2
                    off = 8 - w // 2
                    pk = pooled[k]
                    cx.op("dve", lambda e, cur=cur, x=x, pk=pk, off=off, n_tok=n_tok, o0=o0: e.scalar_tensor_tensor(
                        pk[:, o0:o0 + n_tok], cur[:, off:off + n_tok], 1.0 / w, x[:, 8:8 + n_tok], ALU.mult, ALU.subtract),
                        reads=[cur_tk, xtk], writes=[pooled_tk[k]])
                    for ei, (a0, c0) in enumerate(((0, seg * 16), (n_tok - 8, seg * 16 + 8))):
                        tmp, tmptk = rcp.next()
                        cx.op("dve", lambda e, cur=cur, tmp=tmp, a0=a0, c0=c0, off=off: e.tensor_tensor(
                            tmp[:, 0:8], cur[:, off + a0:off + a0 + 8], inv_t[:, gi, c0:c0 + 8], ALU.mult),
                            reads=[cur_tk, inv_tk], writes=[tmptk])
                        cx.op("dve", lambda e, tmp=tmp, x=x, pk=pk, a0=a0, o0=o0: e.tensor_tensor(
                            pk[:, o0 + a0:o0 + a0 + 8], tmp[:, 0:8], x[:, 8 + a0:8 + a0 + 8], ALU.subtract),
                            reads=[tmptk, xtk, pooled_tk[k]], writes=[pooled_tk[k]])
            for (tok0, N) in (qblocks + ([cblock] if need_ctx else [])):
                for j in range(2):
                    ps, ptk = psS.next()
                    cx.mm(ps[:, :N], [(pw_t[:, 2 * gi + k, j * 128:(j + 1) * 128], pooled[k][:, tok0:tok0 + N]) for k in range(2)],
                          reads=[pw_tk] + pooled_tk, writes=[ptk])
                    st, stk = outst.next()
                    cx.op("act", lambda e, ps=ps, st=st, j=j: e.activation(out=st[:, :N], in_=ps[:, :N], func=AF.Copy,
                                                                           scale=psc_t[:, 2 * gi + j:2 * gi + j + 1]),
                          reads=[ptk, psc_tk], writes=[stk])
                    cx.dma("sp", BT[2 * gi + j, :, tok0:tok0 + N], st[:, :N], reads=[stk])


    def simple_finish(dst_tile):
        def fin(accs, atk, N, tok0):
            r, rtk = rcp.next()
            cx.op("dve", lambda e: e.reciprocal(r[:, :N], accs[2][:, :N]), reads=[atk[2]], writes=[rtk])
            st, stk = outst.next()
            cx.op("dve", lambda e: e.tensor_tensor(st[:, :N], accs[0][:, :N], r[:, :N], ALU.mult),
                  reads=[atk[0], rtk], writes=[stk])
            cx.dma("sp", BT[dst_tile, :, tok0:tok0 + N], st[:, :N], reads=[stk])
        return fin

    with cx.scope():
        MLA_SCALE = 192.0 ** -0.5
        kr = Ring(cx, "att_k", 2, [128, TKEYS], BF16)
        vr = Ring(cx, "att_v", 2, [128, 66, 128], BF16)
        qr = Ring(cx, "att_q", 2, [128, T], BF16)
        krope_t, krope_tk = load_const(cx, "krope", KA[8, :, :], [128, TKEYS], BF16)
        qrope = Ring(cx, "att_qr", 2, [128, T], BF16)
        for h in range(8):
            k_t, k_tk = load(kr, KA[h, :, :])
            v_t, v_tk = vr.next()
            cx.dma("sp", v_t[:, :, 0:128], VA[:, h * 128:(h + 1) * 128].rearrange("(kt p) c -> p kt c", p=128), writes=[v_tk])
            q_t, q_tk = load(qr, QT[h, :, :])
            q2_t, q2_tk = load(qrope, QT[8 + h // 2, :, :])
            pb = 64 * (h % 2)
            fin = simple_finish(8 + h)
            for (tok0, N) in (qblocks + ([cblock] if need_ctx else [])):
                is_c = tok0 >= 2 * NL
                tiles = range(64, 66) if is_c else range(66)
                kl = [([k_t[:, kt * 128:(kt + 1) * 128], krope_t[pb:pb + 64, kt * 128:(kt + 1) * 128]],
                       [v_t[:, kt, 0:128]], [k_tk, v_tk, krope_tk], None) for kt in tiles]
                attn(N, [q_t[:, tok0:tok0 + N], q2_t[pb:pb + 64, tok0:tok0 + N]], [q_tk, q2_tk], kl, MLA_SCALE, 1,
                     lambda accs, atk, N, tok0=tok0: fin(accs, atk, N, tok0))

    with cx.scope():
        WIN_SCALE = 128.0 ** -0.5
        kr = Ring(cx, "wc_k", 2, [128, TT], BF16)
        vr = Ring(cx, "wc_v", 2, [128, 66, 128], BF16)
        qr = Ring(cx, "wq", 2, [128, TT], BF16)
        mk_t, mk_tk = load_const(cx, "wmask", wmask.rearrange("m p q -> p m q"), [128, 10, 512], BF16)
        sk_t, sk_tk = load_const(cx, "sink", sink.partition_broadcast(128), [128, 8])
        cx.op("act", lambda e: e.activation(out=sk_t[:], in_=sk_t[:], func=AF.Exp), reads=[sk_tk], writes=[sk_tk])
        for kv in range(2):
            kw_t, kw_tk = load(kr, KA[9 + kv, :, :])
            vw_t, vw_tk = load(vr, VA[:, 1024 + kv * 128:1024 + (kv + 1) * 128].rearrange("(kt p) c -> p kt c", p=128))
            for gi in range(4):
                h = kv * 4 + gi
                q_t, q_tk = load(qr, QT[12 + h, :, :])

                def finw(accs, atk, N, tok0, h=h):
                    r, rtk = rcp.next()
                    cx.op("dve", lambda e: e.tensor_scalar(r[:, :N], accs[2][:, :N], sk_t[:, h:h + 1], None, ALU.add),
                          reads=[atk[2], sk_tk], writes=[rtk])
                    cx.op("dve", lambda e: e.reciprocal(r[:, :N], r[:, :N]), reads=[rtk], writes=[rtk])
                    st, stk = outst.next()
                    cx.op("dve", lambda e: e.tensor_tensor(st[:, :N], accs[0][:, :N], r[:, :N], ALU.mult),
                          reads=[atk[0], rtk], writes=[stk])
                    cx.dma("sp", BT[16 + h, :, tok0:tok0 + N], st[:, :N], reads=[stk])

                ctx_kl = [([kw_t[:, kt * 128:(kt + 1) * 128]], [vw_t[:, kt, :]], [kw_tk, vw_tk], None) for kt in (64, 65)]
                for (tok0, N) in qblocks:
                    sg = tok0 // NL
                    qb = (tok0 % NL) // 512
                    kl = []
                    for r_ in range(-1, 5):
                        j = qb * 4 + r_
                        if 0 <= j < 32:
                            kt, mi = sg * 32 + j, r_ + 1
                        elif j == -1:
                            kt, mi = (1 - sg) * 32 + 31, 6 + 2 * sg
                        else:
                            kt, mi = (1 - sg) * 32, 7 + 2 * sg
                        kl.append(([kw_t[:, kt * 128:(kt + 1) * 128]], [vw_t[:, kt, :]], [kw_tk, vw_tk], (mk_t[:, mi, :], mk_tk)))
                    attn(N, [q_t[:, tok0:tok0 + N]], [q_tk], kl + ctx_kl, WIN_SCALE, 1,
                         lambda accs, atk, N, tok0=tok0: finw(accs, atk, N, tok0))
                if need_ctx:
                    tok0, N = cblock
                    attn(N, [q_t[:, tok0:tok0 + N]], [q_tk], ctx_kl, WIN_SCALE, 1,
                         lambda accs, atk, N, tok0=tok0: finw(accs, atk, N, tok0))
    with cx.scope():
        DIFF_SCALE = 128.0 ** -0.5
        kr = Ring(cx, "df_k", 2, [128, TT], BF16)
        vr = Ring(cx, "df_v", 2, [128, 66, 256], BF16)
        qr = Ring(cx, "df_q", 2, [128, TT], BF16)
        dl_t, dl_tk = load_const(cx, "dlam", dlam.partition_broadcast(128), [128, 512])
        lam_t = cx.sbuf("lam", [128, 8], F32)
        lam_tk = TK()
        prod = cx.sbuf("lamprod", [128, 256], F32)
        prod_tk = TK()
        for i in range(2):
            cx.op("dve", lambda e, i=i: e.tensor_tensor(prod[:, i * 128:(i + 1) * 128], dl_t[:, 256 * i:256 * i + 128],
                                                        dl_t[:, 256 * i + 128:256 * i + 256], ALU.mult), reads=[dl_tk], writes=[prod_tk])
            cx.op("dve", lambda e, i=i: e.reduce_sum(lam_t[:, i:i + 1], prod[:, i * 128:(i + 1) * 128], axis=AX.X),
                  reads=[prod_tk], writes=[lam_tk])
        cx.op("act", lambda e: e.activation(out=lam_t[:, 2:4], in_=lam_t[:, 0:2], func=AF.Exp), reads=[lam_tk], writes=[lam_tk])
        cx.op("dve", lambda e: e.tensor_tensor(lam_t[:, 4:5], lam_t[:, 3:4], lam_t[:, 2:3], ALU.subtract), reads=[lam_tk], writes=[lam_tk])
        cx.op("dve", lambda e: e.tensor_scalar(lam_t[:, 5:6], lam_t[:, 4:5], -lam_init, None, ALU.add), reads=[lam_tk], writes=[lam_tk])
        dg_t, dg_tk = load_const(cx, "dgfm", dgfm, [128, 2])
        cx.op("dve", lambda e: e.tensor_scalar(dg_t[:], dg_t[:], 1.0 - lam_init, None, ALU.mult), reads=[dg_tk], writes=[dg_tk])
        o_sb = [[cx.sbuf("diff_o%d%d" % (m, j), [128, 512], F32) for j in range(2)] for m in range(2)]
        o_tk = [[TK(), TK()], [TK(), TK()]]
        dsq = Ring(cx, "diff_sq", 2, [128, 512], F32)
        for h in range(4):
            v_t, v_tk = vr.next()
            cx.dma("sp", v_t[:, :, :], VA[:, 1280 + h * 256:1280 + (h + 1) * 256].rearrange("(kt p) c -> p kt c", p=128), writes=[v_tk])
            kq = []
            for m in range(2):
                k_t, k_tk = load(kr, KA[11 + 2 * h + m, :, :])
                q_t, q_tk = load(qr, QT[20 + 2 * h + m, :, :])
                kq.append((k_t, k_tk, q_t, q_tk))
            for (tok0, N) in (qblocks + ([cblock] if need_ctx else [])):
                is_c = tok0 >= 2 * NL
                tiles = range(64, 66) if is_c else range(66)
                for m in range(2):
                    k_t, k_tk, q_t, q_tk = kq[m]
                    kl = [([k_t[:, kt * 128:(kt + 1) * 128]], [v_t[:, kt, 0:128], v_t[:, kt, 128:256]], [k_tk, v_tk], None) for kt in tiles]

                    def find(accs, atk, N, m=m):
                        r, rtk = rcp.next()
                        cx.op("dve", lambda e: e.reciprocal(r[:, :N], accs[2][:, :N]), reads=[atk[2]], writes=[rtk])
                        for j in range(2):
                            cx.op("dve", lambda e, j=j: e.tensor_tensor(o_sb[m][j][:, :N], accs[j][:, :N], r[:, :N], ALU.mult),
                                  reads=[atk[j], rtk], writes=[o_tk[m][j]])

                    attn(N, [q_t[:, tok0:tok0 + N]], [q_tk], kl, DIFF_SCALE, 2, find)
                pss, pstk = psS.next()
                for j in range(2):
                    cx.op("dve", lambda e, j=j: e.scalar_tensor_tensor(o_sb[0][j][:, :N], o_sb[1][j][:, :N], lam_t[:, 5:6],
                                                                       o_sb[0][j][:, :N], ALU.mult, ALU.add),
                          reads=[o_tk[1][j], o_tk[0][j], lam_tk], writes=[o_tk[0][j]])
                    sq, sqtk = dsq.next()
                    cx.op("act", lambda e, j=j, sq=sq: e.activation(out=sq[:, :N], in_=o_sb[0][j][:, :N], func=AF.Square),
                          reads=[o_tk[0][j]], writes=[sqtk])
                    cx.pe_raw(lambda pe, j=j, sq=sq: pe.matmul(pss[:, :N], ones32[:], sq[:, :N], start=(j == 0), stop=(j == 1)),
                              reads=[sqtk, ones_tk], writes=[pstk])
                r, rtk = rcp.next()
                cx.op("act", lambda e: e.activation(out=r[:, :N], in_=pss[:, :N], func=AF.Sqrt, scale=1.0 / 256, bias=EPS_T[0][:, 0:1]),
                      reads=[pstk, EPS_T[1]], writes=[rtk])
                cx.op("dve", lambda e: e.reciprocal(r[:, :N], r[:, :N]), reads=[rtk], writes=[rtk])
                for j in range(2):
                    st, stk = outst.next()
                    cx.op("dve", lambda e, j=j, st=st: e.scalar_tensor_tensor(st[:, :N], o_sb[0][j][:, :N], dg_t[:, j:j + 1],
                                                                              r[:, :N], ALU.mult, ALU.mult),
                          reads=[o_tk[0][j], rtk, dg_tk], writes=[stk])
                    cx.dma("sp", BT[24 + 2 * h + j, :, tok0:tok0 + N], st[:, :N], reads=[stk])


def emit_p3a(cx, l, G):
    need_ctx = (l == 0)
    h_in = G("h_in")
    UT = G("UT")
    BT = G("BT")
    wgate = G("wgate")
    wbr = G("wbr")
    wout = G("wout")
    wmod = G("wmod")
    brow = G("brow")
    cfm = G("cfm")
    h1 = G("h1")
    blocks = BLOCKS_ALL if need_ctx else BLOCKS_ALL[:8]
    psr = PsRing(cx, 8)
    items = []
    for v in range(2):
        for g in range(4):
            items.append((wsrc(wmod, 0, KT, g * 512, 512), KT, 512))
    for _ in blocks:
        for i in range(4):
            for fc in range(4):
                items.append((wsrc(wgate, 0, KT, i * 2048 + fc * 512, 512), KT, 512))
                items.append((wsrc(wbr, i * 1024, 8, fc * 512, 512), 8, 512))
        for fc in range(4):
            items.append((wsrc(wout, 0, KT, fc * 512, 512), KT, 512))
    ws = WStream(cx, items)
    s_t, s_tk, rep_t, rep_tk = prep_silu(cx, cfm, True)
    br_t, br_tk = load_const(cx, "brow", brow.partition_broadcast(128), [128, 2048])
    gt = [cx.sbuf("gt1_%d" % v, [128, 2048], F32) for v in range(2)]
    gt_tk = [TK(), TK()]
    for v in range(2):
        mod_rows(cx, ws, rep_t, rep_tk, br_t, br_tk, 2048, gt[v], gt_tk[v], psr, v)
    uTr = Ring(cx, "uT", 1, [128, KT, 512], BF16)
    acc = cx.sbuf("acc", [128, KT, 512], F32)
    acc_tk = [TK() for _ in range(KT)]
    mT = cx.sbuf("mT", [128, KT, 512], BF16)
    mT_tk = TK()
    btr = Ring(cx, "Bt", 2, [128, 8, 512], BF16)
    sgr = Ring(cx, "sg", 2, [128, 512], F32)
    tmr = Ring(cx, "tm", 2, [128, 512], F32)
    hpr = Ring(cx, "hp", 3, [128, 512], F32)
    yr = Ring(cx, "y1", 2, [128, 512], F32)
    outr = Ring(cx, "o1", 3, [128, 512], F32)
    for (tok0, N, var) in blocks:
        ts = slice(tok0, tok0 + N)
        uT, uT_tk = uTr.next()
        cx.dma("sp", uT[:, :, :N], UT[:, :, ts].rearrange("k p t -> p k t"), writes=[uT_tk])
        for i in range(4):
            Bt, Bt_tk = btr.next()
            cx.dma("sp", Bt[:, :, :N], BT[8 * i:8 * i + 8, :, ts].rearrange("k p t -> p k t"), writes=[Bt_tk])
            for fc in range(4):
                (wg, wgtk), (wo, wotk) = ws.get_many(2)
                for j in range(4):
                    f = fc * 4 + j
                    psg, pgtk = psr.next()
                    cx.mm(psg[:, :N], [(wg[:, k, j * 128:(j + 1) * 128], uT[:, k, :N]) for k in range(KT)],
                          reads=[wgtk, uT_tk], writes=[pgtk])
                    psy, pytk = psr.next()
                    cx.mm(psy[:, :N], [(wo[:, k, j * 128:(j + 1) * 128], Bt[:, k, :N]) for k in range(8)],
                          reads=[wotk, Bt_tk], writes=[pytk])
                    sg, sgtk = sgr.next()
                    cx.op("act", lambda e, sg=sg, psg=psg: e.activation(out=sg[:, :N], in_=psg[:, :N], func=AF.Sigmoid),
                          reads=[pgtk], writes=[sgtk])
                    if i == 0:
                        cx.op("dve", lambda e, sg=sg, psy=psy, f=f: e.tensor_tensor(acc[:, f, :N], psy[:, :N], sg[:, :N], ALU.mult),
                              reads=[pytk, sgtk], writes=[acc_tk[f]])
                    else:
                        tm, tmtk = tmr.next()
                        cx.op("dve", lambda e, sg=sg, psy=psy, tm=tm: e.tensor_tensor(tm[:, :N], psy[:, :N], sg[:, :N], ALU.mult),
                              reads=[pytk, sgtk], writes=[tmtk])
                        if i < 3:
                            cx.op("pool", lambda e, tm=tm, f=f: e.tensor_tensor(acc[:, f, :N], acc[:, f, :N], tm[:, :N], ALU.add),
                                  reads=[tmtk, acc_tk[f]], writes=[acc_tk[f]])
                        else:
                            cx.op("pool", lambda e, tm=tm, f=f: e.tensor_tensor(mT[:, f, :N], acc[:, f, :N], tm[:, :N], ALU.add),
                                  reads=[tmtk, acc_tk[f]], writes=[mT_tk])
        for fc in range(4):
            w, wtk = ws.get()
            for s in range(N // 128):
                ps, ptk = psr.next()
                cx.mm(ps[:, :], [(mT[:, k, s * 128:(s + 1) * 128], w[:, k, :]) for k in range(KT)],
                      reads=[wtk, mT_tk], writes=[ptk])
                r0 = tok0 + s * 128
                hp, hptk = hpr.next()
                cx.dma("sp", hp[:, :], h_in[r0:r0 + 128, fc * 512:(fc + 1) * 512], writes=[hptk])
                y1, y1tk = yr.next()
                cx.op("dve", lambda e, ps=ps, y1=y1, fc=fc: e.tensor_tensor(y1[:, :], ps[:, :], gt[var][:, fc * 512:(fc + 1) * 512], ALU.mult),
                      reads=[ptk, gt_tk[var]], writes=[y1tk])
                o1, o1tk = outr.next()
                cx.op("pool", lambda e, y1=y1, hp=hp, o1=o1: e.tensor_tensor(o1[:, :], y1[:, :], hp[:, :], ALU.add),
                      reads=[y1tk, hptk], writes=[o1tk])
                cx.dma("sp", h1[r0:r0 + 128, fc * 512:(fc + 1) * 512], o1[:, :], reads=[o1tk])


def emit_p3b(cx, l, G):
    need_ctx = (l == 0)
    is_moe = (l % 2 == 1)
    final = (l == 1)
    NE = N_E if is_moe else 1
    h1 = G("h1")
    wg_d = G("wg")
    wu_d = G("wu")
    wd_d = G("wd")
    wmod = G("wmod")
    bmodfm = G("bmodfm")
    brow = G("brow")
    cfm = G("cfm")
    g2fm = G("g2fm")
    ident = G("ident")
    gfin = G("gfin")
    router = G("router")
    routerb = G("routerb")
    h2 = G("h2")
    blocks = BLOCKS_ALL if need_ctx else BLOCKS_ALL[:8]
    make_eps(cx)
    ident_t, ident_tk = make_ident(cx, ident)
    psr = PsRing(cx, 8)
    items = []
    for g in range(8):
        items.append((wsrc(wmod, 0, KT, g * 512, 512), KT, 512))
    for v in range(2):
        for g in range(4):
            items.append((wsrc(wmod, 0, KT, 4096 + g * 512, 512), KT, 512))
    for _ in blocks:
        for e_ in range(NE):
            for hc in range(7):
                for half in range(2):
                    c0 = hc * 1024 + half * 512
                    items.append((wsrc(wg_d, e_ * D, KT, c0, 512), KT, 512))
                    items.append((wsrc(wu_d, e_ * D, KT, c0, 512), KT, 512))
                for ch in range(2):
                    items.append((wsrc(wd_d, e_ * DFF + hc * 1024, 8, ch * 1024, 1024), 8, 1024))
    ws = WStream(cx, items)
    modT = cx.sbuf("modT", [128, 32, 2], F32)
    modT_tk = TK()
    g2_t, g2_tk = load_const(cx, "g2fm", g2fm, [128, KT])
    gt = [cx.sbuf("gt2_%d" % v, [128, 2048], F32) for v in range(2)]
    gt_tk = [TK(), TK()]
    with cx.scope():
        s_t, s_tk, rep_t, rep_tk = prep_silu(cx, cfm, True)
        bm_t, bm_tk = load_const(cx, "bmodfm", bmodfm, [128, 32, 2])
        mod_feature_major(cx, ws, s_t, s_tk, bm_t, bm_tk, list(range(32)), modT, modT_tk, psr)
        br_t, br_tk = load_const(cx, "brow", brow.partition_broadcast(128), [128, 2048])
        for v in range(2):
            mod_rows(cx, ws, rep_t, rep_tk, br_t, br_tk, 2048, gt[v], gt_tk[v], psr, v)
    A_t, A_tk, B_t, B_tk = make_AB(cx, modT, modT_tk, 16, 0, g2_t, g2_tk, "m2")
    if final:
        gf_t, gf_tk = load_const(cx, "gfin", gfin.partition_broadcast(128), [128, 2048])
    if is_moe:
        rt_t = cx.sbuf("router", [128, KT, 8], BF16)
        rt_tk = TK()
        cx.dma("pool", rt_t[:], router.rearrange("(k p) e -> p k e", p=128), writes=[rt_tk])
        rb_t, rb_tk = load_const(cx, "routerb", routerb.partition_broadcast(128), [128, 8])
        gates = cx.sbuf("gates", [128, 4, 8], F32)
        gates_tk = TK()
        gw = cx.sbuf("gatew", [128, 4, 24], F32)
        gw_tk = TK()
    nt = NormT(cx, ident_t, ident_tk, A_t, A_tk, B_t, B_tk, psr)
    uTr = Ring(cx, "uT", 1, [128, KT, 512], BF16)
    acc2 = cx.sbuf("acc2", [128, 4, D], F32)
    acc2_tk = [TK() for _ in range(4)]
    hidr = Ring(cx, "hid", 2, [128, 8, 512], BF16)
    sgr = Ring(cx, "sg", 2, [128, 512], F32)
    st3 = Ring(cx, "fin_st", 2, [128, 4], F32)
    def blk_pro(tok0, N, var):
        nsub = N // 128
        uT, uT_tk = uTr.next()
        nt.run(h1[tok0:tok0 + N, :], N, var, uT, uT_tk)
        if is_moe:
            for s in range(nsub):
                ps, ptk = psr.next()
                cx.mm(ps[:, 0:8], [(uT[:, k, s * 128:(s + 1) * 128], rt_t[:, k, :]) for k in range(KT)],
                      reads=[uT_tk, rt_tk], writes=[ptk])
                lg = gw[:, s, 0:8]
                cx.op("dve", lambda e, ps=ps, lg=lg: e.tensor_tensor(lg, ps[:, 0:8], rb_t[:, :], ALU.add),
                      reads=[ptk, rb_tk], writes=[gw_tk])
                m1 = gw[:, s, 8:9]
                cx.op("dve", lambda e, lg=lg, m1=m1: e.reduce_max(m1, lg, axis=AX.X), reads=[gw_tk], writes=[gw_tk])
                mask1 = gw[:, s, 16:24]
                cx.op("dve", lambda e, lg=lg, m1=m1, mask1=mask1: e.tensor_scalar(mask1, lg, m1, None, ALU.is_equal),
                      reads=[gw_tk], writes=[gw_tk])
                lg2 = gates[:, s, :]
                cx.op("dve", lambda e, lg=lg, mask1=mask1, lg2=lg2: e.scalar_tensor_tensor(lg2, mask1, -1e30, lg, ALU.mult, ALU.add),
                      reads=[gw_tk], writes=[gates_tk])
                m2 = gw[:, s, 9:10]
                cx.op("dve", lambda e, lg2=lg2, m2=m2: e.reduce_max(m2, lg2, axis=AX.X), reads=[gates_tk], writes=[gw_tk])
                d_ = gw[:, s, 10:11]
                cx.op("dve", lambda e, d_=d_, m1=m1, m2=m2: e.tensor_tensor(d_, m2, m1, ALU.subtract), reads=[gw_tk], writes=[gw_tk])
                e2 = gw[:, s, 11:12]
                cx.op("act", lambda e, d_=d_, e2=e2: e.activation(out=e2, in_=d_, func=AF.Exp), reads=[gw_tk], writes=[gw_tk])
                w1 = gw[:, s, 12:13]
                cx.op("dve", lambda e, e2=e2, w1=w1: e.tensor_scalar(w1, e2, 1.0, None, ALU.add), reads=[gw_tk], writes=[gw_tk])
                cx.op("dve", lambda e, w1=w1: e.reciprocal(w1, w1), reads=[gw_tk], writes=[gw_tk])
                w2 = gw[:, s, 13:14]
                cx.op("dve", lambda e, e2=e2, w1=w1, w2=w2: e.tensor_tensor(w2, e2, w1, ALU.mult), reads=[gw_tk], writes=[gw_tk])
                cx.op("dve", lambda e, lg2=lg2, m2=m2, w2=w2: e.tensor_scalar(lg2, lg2, m2, w2, ALU.is_equal, ALU.mult),
                      reads=[gw_tk, gates_tk], writes=[gates_tk])
                cx.op("dve", lambda e, lg2=lg2, mask1=mask1, w1=w1: e.scalar_tensor_tensor(lg2, mask1, w1, lg2, ALU.mult, ALU.add),
                      reads=[gw_tk, gates_tk], writes=[gates_tk])
        return uT, uT_tk, nsub

    def blk_ffn(tok0, N, var, uT, uT_tk, nsub):
        for e_ in range(NE):
            for hc in range(7):
                hid, hid_tk = hidr.next()
                for half in range(2):
                    (wg, wgtk), (wu, wutk) = ws.get_many(2)
                    for j in range(4):
                        psg, pgtk = psr.next()
                        cx.mm(psg[:, :N], [(wg[:, k, j * 128:(j + 1) * 128], uT[:, k, :N]) for k in range(KT)],
                              reads=[wgtk, uT_tk], writes=[pgtk])
                        psu, putk = psr.next()
                        cx.mm(psu[:, :N], [(wu[:, k, j * 128:(j + 1) * 128], uT[:, k, :N]) for k in range(KT)],
                              reads=[wutk, uT_tk], writes=[putk])
                        sg, sgtk = sgr.next()
                        cx.op("act", lambda e, sg=sg, psg=psg: e.activation(out=sg[:, :N], in_=psg[:, :N], func=AF.Silu),
                              reads=[pgtk], writes=[sgtk])
                        cx.op("dve", lambda e, sg=sg, psu=psu, hid=hid, half=half, j=j: e.tensor_tensor(
                            hid[:, half * 4 + j, :N], psu[:, :N], sg[:, :N], ALU.mult), reads=[putk, sgtk], writes=[hid_tk])
                for ch in range(2):
                    wd, wdtk = ws.get()
                    for s in range(nsub):
                        for cc in range(2):
                            ps, ptk = psr.next()
                            cx.mm(ps[:, :], [(hid[:, k, s * 128:(s + 1) * 128], wd[:, k, cc * 512:(cc + 1) * 512]) for k in range(8)],
                                  reads=[wdtk, hid_tk], writes=[ptk])
                            dst = acc2[:, s, ch * 1024 + cc * 512:ch * 1024 + (cc + 1) * 512]
                            first = (e_ == 0 and hc == 0)
                            if not is_moe:
                                if first:
                                    cx.op("act", lambda e, ps=ps, dst=dst: e.activation(out=dst, in_=ps[:, :], func=AF.Copy),
                                          reads=[ptk], writes=[acc2_tk[s]])
                                else:
                                    cx.op("dve", lambda e, ps=ps, dst=dst: e.tensor_tensor(dst, ps[:, :], dst, ALU.add),
                                          reads=[ptk, acc2_tk[s]], writes=[acc2_tk[s]])
                            else:
                                gsc = gates[:, s, e_:e_ + 1]
                                if first:
                                    cx.op("dve", lambda e, ps=ps, dst=dst, gsc=gsc: e.tensor_scalar(dst, ps[:, :], gsc, None, ALU.mult),
                                          reads=[ptk, gates_tk], writes=[acc2_tk[s]])
                                else:
                                    cx.op("dve", lambda e, ps=ps, dst=dst, gsc=gsc: e.scalar_tensor_tensor(dst, ps[:, :], gsc, dst, ALU.mult, ALU.add),
                                          reads=[ptk, gates_tk, acc2_tk[s]], writes=[acc2_tk[s]])

    def blk_epi(tok0, N, var, nsub):
        for s in range(nsub):
            r0 = tok0 + s * 128
            ht, htk = nt.hr.next()
            cx.dma("sp", ht[:], h1[r0:r0 + 128, :], writes=[htk])
            cx.op("dve", lambda e, s=s: e.tensor_tensor(acc2[:, s, :], acc2[:, s, :], gt[var][:, :], ALU.mult),
                  reads=[acc2_tk[s], gt_tk[var]], writes=[acc2_tk[s]])
            cx.op("pool", lambda e, s=s, ht=ht: e.tensor_tensor(ht[:], acc2[:, s, :], ht[:], ALU.add),
                  reads=[acc2_tk[s], htk], writes=[htk])
            if final:
                st, stk = st3.next()
                cx.op("act", lambda e, ht=ht, st=st: e.activation(out=nt.junk[:], in_=ht[:], func=AF.Square, accum_out=st[:, 0:1]),
                      reads=[htk], writes=[nt.junk_tk, stk])
                cx.op("act", lambda e, st=st: e.activation(out=st[:, 1:2], in_=st[:, 0:1], func=AF.Sqrt, scale=1.0 / D, bias=EPS_T[0][:, 0:1]),
                      reads=[stk, EPS_T[1]], writes=[stk])
                cx.op("dve", lambda e, st=st: e.reciprocal(st[:, 2:3], st[:, 1:2]), reads=[stk], writes=[stk])
                cx.op("dve", lambda e, ht=ht, st=st: e.scalar_tensor_tensor(ht[:], ht[:], st[:, 2:3], gf_t[:, :], ALU.mult, ALU.mult),
                      reads=[htk, stk, gf_tk], writes=[htk])
            cx.dma("sp", h2[r0:r0 + 128, :], ht[:], reads=[htk], final=final)

    st_ = blk_pro(*blocks[0])
    for bi, (tok0, N, var) in enumerate(blocks):
        uT, uT_tk, nsub = st_
        blk_ffn(tok0, N, var, uT, uT_tk, nsub)
        if bi + 1 < len(blocks):
            st_ = blk_pro(*blocks[bi + 1])
        blk_epi(tok0, N, var, nsub)


def build_fused():
    cx = Ctx()
    d = {}

    def ext(name, shape, dt=F32):
        d[name] = cx.dram(name, shape, dt, "ExternalInput")

    def scr(name, shape, dt):
        d[name] = cx.dram(name, shape, dt, "Internal")

    ext("h0", [TT, D])
    ext("cfm", [128, KT, 2])
    ext("ident", [128, 128])
    ext("rope", [4, 128, TT])
    ext("wmask", [10, 128, 512], BF16)
    ext("invcnt", [128, 4, 48])
    ext("pflag", [128, 4])
    ext("gfin", [1, 2048])
    ext("router", [D, 8])
    ext("routerb", [1, 8])
    for l in range(2):
        sfx = "_%d" % l
        ext("w1" + sfx, [D, W1_COLS])
        ext("wkvb" + sfx, [512, 2048])
        ext("wmod1" + sfx, [D, 4096])
        ext("bmodfm1" + sfx, [128, 32, 2])
        ext("g1fm" + sfx, [128, KT])
        ext("kvgfm" + sfx, [128, 4])
        ext("poolw" + sfx, [4, 256, 256])
        ext("pscfm" + sfx, [128, 8])
        ext("dlam" + sfx, [1, 512])
        ext("dgfm" + sfx, [128, 2])
        ext("sink" + sfx, [1, 8])
        ext("wgate" + sfx, [D, 8192])
        ext("wbr" + sfx, [4096, D])
        ext("wout" + sfx, [D, D])
        ext("wmodg1" + sfx, [D, 2048])
        ext("brow1" + sfx, [1, 2048])
        NE = N_E if l == 1 else 1
        ext("wg" + sfx, [NE * D, DFF])
        ext("wu" + sfx, [NE * D, DFF])
        ext("wd" + sfx, [NE * DFF, D])
        ext("wmod2" + sfx, [D, 6144])
        ext("bmodfm2" + sfx, [128, 32, 2])
        ext("brow2" + sfx, [1, 2048])
        ext("g2fm" + sfx, [128, KT])
    scr("UT", [KT, 128, TT], BF16)
    scr("QT", [NQT, 128, TT], BF16)
    scr("KT", [NKT, 128, TT], BF16)
    scr("V", [TT, VW], BF16)
    scr("AT", [8, 128, TT], BF16)
    scr("BT", [32, 128, TT], BF16)
    scr("h1", [TT, D], F32)
    scr("h2", [TT, D], F32)
    d["out"] = cx.dram("out", [NL, D], F32, "ExternalOutput")

    def mkG(l, alias):
        def G(name):
            n = alias.get(name, name)
            nl = n + "_%d" % l
            return d[nl] if nl in d else d[n]
        return G

    for l in range(2):
        hin = "h0" if l == 0 else "h2"
        with cx.scope():
            emit_p1(cx, l, mkG(l, dict(h_in=hin, wmod="wmod1", bmodfm="bmodfm1")))
        with cx.scope():
            emit_p2(cx, l, mkG(l, {}))
        with cx.scope():
            emit_p3a(cx, l, mkG(l, dict(h_in=hin, wmod="wmodg1", brow="brow1")))
        with cx.scope():
            emit_p3b(cx, l, mkG(l, dict(wmod="wmod2", bmodfm="bmodfm2", brow="brow2", h2=("h2" if l == 0 else "out"))))
    cx.finish()
    return cx.nc


def rope_tables(half):
    t = np.concatenate([np.arange(NL) + half * NL, np.arange(NL) + (1 - half) * NL])
    row = (t // 64).astype(np.float32)
    col = (t % 64).astype(np.float32)
    out = np.zeros((4, 128, TT), np.float32)
    for ti, dd in enumerate((64, 128)):
        q = dd // 4
        p = np.arange(128) % dd
        quarter = p // q
        i = (p % q).astype(np.float32)
        inv = (np.float32(10000.0) ** (-i / np.float32(q))).astype(np.float32)
        pos = np.where((quarter < 2)[:, None], row[None, :], col[None, :]).astype(np.float32)
        ang = (pos * inv[:, None]).astype(np.float32)
        sgn = np.where(quarter % 2 == 0, -1.0, 1.0).astype(np.float32)[:, None]
        out[2 * ti, :, :2 * NL] = np.cos(ang)
        out[2 * ti + 1, :, :2 * NL] = np.sin(ang) * sgn
        out[2 * ti, :, 2 * NL:] = 1.0
    return out


def _mask_tiles(half):
    k = np.arange(128)[:, None]
    q = np.arange(512)[None, :]
    m = np.zeros((10, 128, 512), np.float32)
    for r in range(-1, 5):
        m[r + 1] = (np.abs(q - k - 128 * r) <= 128)
    if half == 1:
        m[6] = m[0]
        m[9] = m[5]
    else:
        m[7] = m[5]
        m[8] = m[0]
    return m.astype(NPBF)


def _invcnt(half):
    out = np.zeros((128, 4, 48), np.float32)
    segs = ((2 * NL, half * NL, NL), (2 * NL, (1 - half) * NL, NL), (NCX, 0, NCX))
    for gi, w in enumerate((2, 4, 8, 16)):
        for seg, (n, base, ntok) in enumerate(segs):
            for ei, a0 in enumerate((0, ntok - 8)):
                t = base + a0 + np.arange(8)
                lo = np.clip(t - w // 2, 0, n)
                hi = np.clip(t - w // 2 + w, 0, n)
                out[:, gi, seg * 16 + ei * 8:seg * 16 + ei * 8 + 8] = (1.0 / (hi - lo).astype(np.float32))[None, :]
    return out


def _pflag(half):
    f = np.zeros((128, 4), np.float32)
    f[:, 0] = float(half == 1)
    f[:, 1] = float(half == 0)
    f[:, 2] = float(half == 0)
    f[:, 3] = float(half == 1)
    return f


def _cfm(inp, b):
    return np.ascontiguousarray(np.stack([fm(inp["c"][b]), fm(inp["c_ctx"])], axis=2))


def shared_inputs(inp):
    sh = dict(ident=IDENT, gfin=np.ascontiguousarray(inp["final_norm_g"].reshape(1, 2048)),
              router=np.ascontiguousarray(inp["moe_router"][0]), routerb=np.ascontiguousarray(inp["moe_router_b"][0].reshape(1, 8)))
    for l in range(2):
        s = "_%d" % l
        bm = inp["b_mod"][l]
        wm = inp["w_mod"][l]
        sh["w1" + s] = np.ascontiguousarray(inp["w_in"][l][:, _W1_IDX])
        sh["wkvb" + s] = np.ascontiguousarray(inp["mla_w_kv_b"][l][:, _KVB_IDX])
        sh["wmod1" + s] = np.ascontiguousarray(wm[:, 0:4096])
        sh["bmodfm1" + s] = np.ascontiguousarray(np.repeat(fm(bm[0:4096])[:, :, None], 2, axis=2))
        sh["g1fm" + s] = fm(inp["norm1_g"][l])
        sh["kvgfm" + s] = fm(inp["mla_kv_norm_g"][l])
        sh["poolw" + s] = np.ascontiguousarray(inp["pool_w"][l])
        sh["pscfm" + s] = fm(inp["pool_scale"][l])
        sh["dlam" + s] = np.ascontiguousarray(inp["diff_lambda"][l].reshape(1, 512))
        sh["dgfm" + s] = fm(inp["diff_subln_g"][l])
        sh["sink" + s] = np.ascontiguousarray(inp["win_sink"][l].reshape(1, 8))
        sh["wgate" + s] = np.ascontiguousarray(inp["w_in"][l][:, GATE0:])
        sh["wbr" + s] = np.concatenate([inp["pool_out"][l], inp["mla_out"][l], inp["win_out"][l], inp["diff_out"][l]], axis=0)
        sh["wout" + s] = np.ascontiguousarray(inp["w_out"][l])
        sh["wmodg1" + s] = np.ascontiguousarray(wm[:, 4096:6144])
        sh["brow1" + s] = np.ascontiguousarray(bm[4096:6144].reshape(1, 2048))
        sh["wmod2" + s] = np.ascontiguousarray(wm[:, 6144:12288])
        sh["bmodfm2" + s] = np.ascontiguousarray(np.repeat(fm(bm[6144:10240])[:, :, None], 2, axis=2))
        sh["brow2" + s] = np.ascontiguousarray(bm[10240:12288].reshape(1, 2048))
        sh["g2fm" + s] = fm(inp["norm2_g"][l])
    sh["wg_0"], sh["wu_0"], sh["wd_0"] = inp["ffn_w_gate"][0], inp["ffn_w_up"][0], inp["ffn_w_down"][0]
    sh["wg_1"] = inp["moe_w_gate"][0].reshape(N_E * D, DFF)
    sh["wu_1"] = inp["moe_w_up"][0].reshape(N_E * D, DFF)
    sh["wd_1"] = inp["moe_w_down"][0].reshape(N_E * DFF, D)
    return sh


_NC = [None]


def kernel(**inp):
    inp = {k: np.asarray(v) for k, v in inp.items()}
    sh = shared_inputs(inp)
    maps = []
    for core in range(8):
        b, half = core // 2, core % 2
        m = dict(sh)
        xb = inp["x"][b]
        m["h0"] = np.ascontiguousarray(np.concatenate(
            [xb[half * NL:(half + 1) * NL], xb[(1 - half) * NL:(2 - half) * NL], inp["ctx"][b]], axis=0))
        m["cfm"] = _cfm(inp, b)
        m["rope"] = rope_tables(half)
        m["wmask"] = _mask_tiles(half)
        m["invcnt"] = _invcnt(half)
        m["pflag"] = _pflag(half)
        maps.append(m)
    if _NC[0] is None:
        _NC[0] = build_fused()
    res = run_bass_kernel_spmd(_NC[0], maps, core_ids=list(range(8)))
    out = np.empty((4, 2 * NL, D), np.float32)
    for core in range(8):
        out[core // 2, (core % 2) * NL:(core % 2 + 1) * NL] = np.asarray(res.results[core]["out"])
    return out
```
